# Optimizing a Trainium2 kernel written in Bass

```python
import jax, jax.numpy as jnp
from jax import lax
import numpy as np

D_MODEL = 1024
BATCH = 8
SEQ = 8192
DEPTH = 2
DEC_BATCH = 4
DEC_SEQ = 4096
PAST_LEN = 128

GRID_W = 64
PLE_DIM = 256
GLA_HEADS = 4
GLA_DK = 64
GLA_DV = 128
GLA_RANK = 16
GLA_NORMALIZER = 16.0
GLA_CHUNK = 64
NA_HEADS = 8
NA_DH = 64
NA_KH = 8
NA_KW = 16
GLA_QK = GLA_HEADS * GLA_DK
GLA_V = GLA_HEADS * GLA_DV
NA_W = NA_HEADS * NA_DH
MIX_W = GLA_V + NA_W
IN_W = 2 * GLA_QK + 2 * GLA_V + 2 * GLA_RANK + 3 * NA_W
N_EXPERTS = 32
TOP_K = 4
D_EXPERT = D_MODEL
SWIGLU_ALPHA = 1.702
SWIGLU_LIMIT = 7.0
MOE_BLOCK = 256
DEEP_ALPHA = (2 * DEPTH) ** 0.25
DEEP_BETA = (8 * DEPTH) ** -0.25
LN_EPS = 1e-5
RMS_EPS = 1e-6

kernel_name = 'hybrid_gla_natten_moe_encoder'


def _layer_norm(x, g, b):
    xf = x.astype(jnp.float32)
    mu = jnp.mean(xf, axis=-1, keepdims=True)
    var = jnp.mean(jnp.square(xf - mu), axis=-1, keepdims=True)
    return ((xf - mu) * lax.rsqrt(var + LN_EPS) * g + b).astype(x.dtype)


def _heads(a, h):
    B, T, _ = a.shape
    return a.reshape(B, T, h, -1).transpose(0, 2, 1, 3)


def _gla_direction(q, k, v, logg, strict):
    B, H, T, dk = q.shape
    dv = v.shape[-1]
    C = GLA_CHUNK
    n = T // C

    def chunks(a):
        return a.reshape(B, H, n, C, a.shape[-1]).transpose(2, 0, 1, 3, 4)

    idx = jnp.arange(C)
    mask = (idx[:, None] > idx[None, :]) if strict else (idx[:, None] >= idx[None, :])

    def step(S, inp):
        qi, ki, vi, gi = inp
        b = jnp.cumsum(gi, axis=2)
        diff = b[:, :, :, None, :] - b[:, :, None, :, :]
        decay = jnp.exp(jnp.where(mask[:, :, None], diff, -jnp.inf))
        A = jnp.einsum('bhid,bhjd,bhijd->bhij', qi, ki, decay)
        o = jnp.einsum('bhij,bhjv->bhiv', A, vi) + jnp.einsum('bhid,bhdv->bhiv', qi * jnp.exp(b), S)
        b_last = b[:, :, -1:, :]
        S = jnp.exp(b_last[:, :, 0, :])[..., None] * S + jnp.einsum('bhjd,bhjv->bhdv', ki * jnp.exp(b_last - b), vi)
        return S, o

    S0 = jnp.zeros((B, H, dk, dv), jnp.float32)
    _, o = lax.scan(step, S0, (chunks(q), chunks(k), chunks(v), chunks(logg)))
    return o.transpose(1, 2, 0, 3, 4).reshape(B, H, T, dv)


def _gla_group(q, k, v, g_out, lr_f, lr_b, w_gk_f, b_gk_f, w_gk_b, b_gk_b, norm_g):
    B, T, _ = q.shape
    f32 = jnp.float32
    qh = _heads(q, GLA_HEADS).astype(f32) * GLA_DK ** -0.5
    kh = _heads(k, GLA_HEADS).astype(f32)
    vh = _heads(v, GLA_HEADS).astype(f32)
    lg_f = _heads(jax.nn.log_sigmoid((lr_f @ w_gk_f + b_gk_f).astype(f32)) / GLA_NORMALIZER, GLA_HEADS)
    lg_b = _heads(jax.nn.log_sigmoid((lr_b @ w_gk_b + b_gk_b).astype(f32)) / GLA_NORMALIZER, GLA_HEADS)
    flip = lambda a: jnp.flip(a, axis=2)
    o_f = _gla_direction(qh, kh, vh, lg_f, strict=False)
    o_b = flip(_gla_direction(flip(qh), flip(kh), flip(vh), flip(lg_b), strict=True))
    o = (o_f + o_b).transpose(0, 2, 1, 3)
    o = o * lax.rsqrt(jnp.mean(jnp.square(o), axis=-1, keepdims=True) + RMS_EPS)
    o = o * norm_g.astype(f32).reshape(GLA_HEADS, GLA_DV)
    o = o.reshape(B, T, GLA_V) * jax.nn.silu(g_out.astype(f32))
    return o.astype(q.dtype)


def _na_group(q, k, v, rpb):
    B, T, _ = q.shape
    rows = T // GRID_W
    kh = min(NA_KH, rows)

    def grid(a):
        return a.reshape(B, rows, GRID_W, NA_HEADS, NA_DH).transpose(1, 0, 3, 2, 4)

    qg = grid(q) * NA_DH ** -0.5
    kg = grid(k)
    vg = grid(v)
    col = jnp.arange(GRID_W)
    col_idx = jnp.clip(col - NA_KW // 2, 0, GRID_W - NA_KW)[:, None] + jnp.arange(NA_KW)[None, :]
    col_off = col_idx - col[:, None] + (NA_KW - 1)
    row = jnp.arange(rows)
    row_start = jnp.clip(row - kh // 2, 0, rows - kh)

    def row_block(inp):
        qr, r, rs = inp
        kr = lax.dynamic_slice_in_dim(kg, rs, kh, axis=0)[:, :, :, col_idx, :]
        vr = lax.dynamic_slice_in_dim(vg, rs, kh, axis=0)[:, :, :, col_idx, :]
        s = jnp.einsum('bhcd,abhcwd->bhcaw', qr, kr).astype(jnp.float32)
        row_off = rs + jnp.arange(kh) - r + (NA_KH - 1)
        bias = rpb[:, row_off][:, :, col_off].transpose(0, 2, 1, 3)
        s = s + bias.astype(jnp.float32)[None]
        p = jax.nn.softmax(s.reshape(B, NA_HEADS, GRID_W, kh * NA_KW), axis=-1)
        p = p.reshape(B, NA_HEADS, GRID_W, kh, NA_KW).astype(vr.dtype)
        return jnp.einsum('bhcaw,abhcwd->bhcd', p, vr)

    o = lax.map(row_block, (qg, row, row_start))
    return o.transpose(1, 0, 3, 2, 4).reshape(B, T, NA_W)


def _moe(x, w_router, b_router, w_gu, b_gu, w_down, b_down):
    B, T, D = x.shape
    N = B * T
    f32 = jnp.float32
    xf = x.reshape(N, D)
    logits = xf.astype(f32) @ w_router.astype(f32) + b_router.astype(f32)
    top_v, top_e = lax.top_k(logits, TOP_K)
    probs = jax.nn.softmax(top_v, axis=-1).astype(x.dtype)
    NK = N * TOP_K
    C = MOE_BLOCK
    flat_e = top_e.reshape(NK)
    flat_t = jnp.arange(NK, dtype=jnp.int32) // TOP_K
    flat_p = probs.reshape(NK)
    order = jnp.argsort(flat_e)
    se = flat_e[order]
    counts = jnp.bincount(flat_e, length=N_EXPERTS)
    padded = (counts + C - 1) // C * C
    pad_end = jnp.cumsum(padded)
    pad_start = pad_end - padded
    start = jnp.cumsum(counts) - counts
    dest = pad_start[se] + jnp.arange(NK, dtype=jnp.int32) - start[se]
    n_blocks = (NK + N_EXPERTS * (C - 1) + C - 1) // C
    L = n_blocks * C
    tok = jnp.full((L,), N, jnp.int32).at[dest].set(flat_t[order])
    wts = jnp.zeros((L,), x.dtype).at[dest].set(flat_p[order])
    blk_e = jnp.minimum(jnp.searchsorted(pad_end, jnp.arange(n_blocks) * C, side='right'), N_EXPERTS - 1)
    x_pad = jnp.concatenate([xf, jnp.zeros((1, D), x.dtype)], axis=0)

    def step(acc, inp):
        t, w, e = inp
        h = x_pad[t] @ w_gu[e] + b_gu[e]
        gate = jnp.minimum(h[:, :D_EXPERT], SWIGLU_LIMIT)
        up = jnp.clip(h[:, D_EXPERT:], -SWIGLU_LIMIT, SWIGLU_LIMIT)
        glu = gate * jax.nn.sigmoid(gate * SWIGLU_ALPHA)
        y = ((up + 1.0) * glu) @ w_down[e] + b_down[e]
        return acc.at[t].add(y * w[:, None]), None

    acc0 = jnp.zeros((N + 1, D), x.dtype)
    acc, _ = lax.scan(step, acc0, (tok.reshape(n_blocks, C), wts.reshape(n_blocks, C), blk_e))
    return acc[:N].reshape(B, T, D)


def _layer(x, p, w_in, w_gk_f, b_gk_f, w_gk_b, b_gk_b, gla_norm_g, rpb, w_out, ln1_g, ln1_b,
           w_router, b_router, w_gu, b_gu, w_down, b_down, w_ple_proj, w_ple_gate, b_ple_gate, ln2_g, ln2_b):
    z = x @ w_in
    sizes = (GLA_QK, GLA_QK, GLA_V, GLA_V, GLA_RANK, GLA_RANK, NA_W, NA_W)
    cuts = [int(c) for c in np.cumsum(sizes)]
    q_g, k_g, v_g, g_g, lr_f, lr_b, q_n, k_n, v_n = jnp.split(z, cuts, axis=-1)
    o_gla = _gla_group(q_g, k_g, v_g, g_g, lr_f, lr_b, w_gk_f, b_gk_f, w_gk_b, b_gk_b, gla_norm_g)
    o_na = _na_group(q_n, k_n, v_n, rpb)
    mix = jnp.concatenate([o_gla, o_na], axis=-1) @ w_out
    x = _layer_norm(DEEP_ALPHA * x + mix, ln1_g, ln1_b)
    r = DEEP_ALPHA * x + _moe(x, w_router, b_router, w_gu, b_gu, w_down, b_down)
    u = (p @ w_ple_proj) * jax.nn.sigmoid(r @ w_ple_gate + b_ple_gate)
    return _layer_norm(r + u, ln2_g, ln2_b)


def _trunk(x, p, emb_ln_g, emb_ln_b, w_in, w_gk_f, b_gk_f, w_gk_b, b_gk_b, gla_norm_g, rpb, w_out,
           ln1_g, ln1_b, w_router, b_router, w_gu, b_gu, w_down, b_down, w_ple_proj, w_ple_gate,
           b_ple_gate, ln2_g, ln2_b):
    x = _layer_norm(x, emb_ln_g, emb_ln_b)
    for i in range(DEPTH):
        x = _layer(x, p[i], w_in[i], w_gk_f[i], b_gk_f[i], w_gk_b[i], b_gk_b[i], gla_norm_g[i], rpb[i],
                   w_out[i], ln1_g[i], ln1_b[i], w_router[i], b_router[i], w_gu[i], b_gu[i], w_down[i],
                   b_down[i], w_ple_proj[i], w_ple_gate[i], b_ple_gate[i], ln2_g[i], ln2_b[i])
    return x


def setup_inputs(seed: int = 0) -> dict:
    key = jax.random.key(seed)
    ks = jax.random.split(key, 28)
    f32 = jnp.float32

    def nrm(k, shape, scale):
        return jax.random.normal(k, shape, f32) * scale

    L = DEPTH
    E = N_EXPERTS
    return {
        'x_prompt': nrm(ks[0], (BATCH, SEQ, D_MODEL), 1.0),
        'x_sample': nrm(ks[1], (DEC_BATCH, DEC_SEQ, D_MODEL), 1.0),
        'p_prompt': nrm(ks[2], (DEPTH, BATCH, SEQ, PLE_DIM), 1.0),
        'p_sample': nrm(ks[3], (DEPTH, DEC_BATCH, DEC_SEQ, PLE_DIM), 1.0),
        'emb_ln_g': 1.0 + nrm(ks[4], (D_MODEL,), 0.02),
        'emb_ln_b': nrm(ks[5], (D_MODEL,), 0.02),
        'w_in': nrm(ks[6], (L, D_MODEL, IN_W), D_MODEL ** -0.5),
        'w_gk_f': nrm(ks[7], (L, GLA_RANK, GLA_QK), GLA_RANK ** -0.5),
        'b_gk_f': nrm(ks[8], (L, GLA_QK), 0.5),
        'w_gk_b': nrm(ks[9], (L, GLA_RANK, GLA_QK), GLA_RANK ** -0.5),
        'b_gk_b': nrm(ks[10], (L, GLA_QK), 0.5),
        'gla_norm_g': 1.0 + nrm(ks[11], (L, GLA_V), 0.02),
        'rpb': nrm(ks[12], (L, NA_HEADS, 2 * NA_KH - 1, 2 * NA_KW - 1), 0.1),
        'w_out': nrm(ks[13], (L, MIX_W, D_MODEL), MIX_W ** -0.5 * DEEP_BETA),
        'ln1_g': 1.0 + nrm(ks[14], (L, D_MODEL), 0.02),
        'ln1_b': nrm(ks[15], (L, D_MODEL), 0.02),
        'w_router': nrm(ks[16], (L, D_MODEL, E), D_MODEL ** -0.5),
        'b_router': nrm(ks[17], (L, E), 0.01),
        'w_gu': nrm(ks[18], (L, E, D_MODEL, 2 * D_EXPERT), D_MODEL ** -0.5),
        'b_gu': nrm(ks[19], (L, E, 2 * D_EXPERT), 0.02),
        'w_down': nrm(ks[20], (L, E, D_EXPERT, D_MODEL), D_EXPERT ** -0.5 * DEEP_BETA),
        'b_down': nrm(ks[21], (L, E, D_MODEL), 0.02),
        'w_ple_proj': nrm(ks[22], (L, PLE_DIM, D_MODEL), PLE_DIM ** -0.5 * DEEP_BETA),
        'w_ple_gate': nrm(ks[23], (L, D_MODEL, D_MODEL), D_MODEL ** -0.5),
        'b_ple_gate': nrm(ks[24], (L, D_MODEL), 0.02),
        'ln2_g': 1.0 + nrm(ks[25], (L, D_MODEL), 0.02),
        'ln2_b': nrm(ks[26], (L, D_MODEL), 0.02),
    }


def reference(x_prompt, x_sample, p_prompt, p_sample, emb_ln_g, emb_ln_b, w_in, w_gk_f, b_gk_f, w_gk_b,
              b_gk_b, gla_norm_g, rpb, w_out, ln1_g, ln1_b, w_router, b_router, w_gu, b_gu, w_down, b_down,
              w_ple_proj, w_ple_gate, b_ple_gate, ln2_g, ln2_b):
    y_prompt = _trunk(x_prompt, p_prompt, emb_ln_g, emb_ln_b, w_in, w_gk_f, b_gk_f, w_gk_b, b_gk_b,
                      gla_norm_g, rpb, w_out, ln1_g, ln1_b, w_router, b_router, w_gu, b_gu, w_down, b_down,
                      w_ple_proj, w_ple_gate, b_ple_gate, ln2_g, ln2_b)
    y_sample = _trunk(x_sample, p_sample, emb_ln_g, emb_ln_b, w_in, w_gk_f, b_gk_f, w_gk_b, b_gk_b,
                      gla_norm_g, rpb, w_out, ln1_g, ln1_b, w_router, b_router, w_gu, b_gu, w_down, b_down,
                      w_ple_proj, w_ple_gate, b_ple_gate, ln2_g, ln2_b)
    return (y_prompt, y_sample)
```

```python
import numpy as np
import ml_dtypes
from contextlib import ExitStack
import concourse.bass as bass
import concourse.mybir as mybir
from concourse.bass_utils import run_bass_kernel_spmd

F32 = mybir.dt.float32
BF16 = mybir.dt.bfloat16
I32 = mybir.dt.int32
U32 = mybir.dt.uint32
ALU = mybir.AluOpType
AF = mybir.ActivationFunctionType
AX = mybir.AxisListType

D = 1024
PLE = 256
NE = 32
TOPK = 4
GRID_W = 64
INW = 3104
C_QG, C_KG, C_VG, C_GG, C_LRF, C_LRB, C_QN, C_KN, C_VN = 0, 256, 512, 1024, 1536, 1552, 1568, 2080, 2592
SW_ALPHA = 1.702
SW_LIM = 7.0
LN_EPS = 1e-5
RMS_EPS = 1e-6
NEG = -30000.0


class Cfg:
    def __init__(self, Tp=8192, Ts=4096, depth=2, cap=2048, debug=False, serial_scatter=False):
        self.Tp, self.Ts, self.depth, self.cap, self.debug = Tp, Ts, depth, cap, debug
        self.serial_scatter = serial_scatter
        self.N = Tp + Ts
        self.seqs = [(0, Tp), (Tp, Ts)]
        self.alpha = (2 * depth) ** 0.25
        assert Tp % 512 == 0 and Ts % 512 == 0 and cap % 128 == 0


class Tk:
    __slots__ = ("t", "w", "r")

    def __init__(self, t):
        self.t = t
        self.w = None
        self.r = {}

    def __getitem__(self, key):
        return self.t[key]


class Ring:
    def __init__(self, items):
        self.items = items
        self.i = 0

    def next(self):
        it = self.items[self.i % len(self.items)]
        self.i += 1
        return it


class Kern:
    ND = 8

    def __init__(self, nc, es):
        self.nc = nc
        self.es = es
        self.E = {"pe": nc.tensor, "act": nc.scalar, "dve": nc.vector, "pool": nc.gpsimd, "sp": nc.sync}
        self.semo = {}
        self.cnt = {}
        for e in self.E:
            self.semo[("c", e)] = es.enter_context(nc.semaphore("prog_" + e))
            self.cnt[("c", e)] = 0
        self.seen = {e: {} for e in self.E}
        self.dq = {}
        for q in ("sp", "pool", "act"):
            keys = []
            for i in range(self.ND):
                key = ("d", q, i)
                self.semo[key] = es.enter_context(nc.semaphore(f"dma_{q}{i}"))
                self.cnt[key] = 0
                keys.append(key)
            self.dq[q] = Ring(keys)
        self.same_sync = True
        self.ninst = 0
        self.yield_ = None
        self.stream = None
        self.last_e = {}

    def sb(self, name, shape, dt):
        self.uid = getattr(self, "uid", 0) + 1
        return Tk(self.es.enter_context(self.nc.sbuf_tensor(f"{name}_u{self.uid}", shape, dt)))

    def ring(self, name, shape, dt, n):
        return Ring([self.sb(f"{name}{i}", shape, dt) for i in range(n)])

    def wait(self, e, tok):
        key, val = tok
        if key == ("c", e):
            if e == "pe" or e == "sp" or not self.same_sync:
                return
        if self.seen[e].get(key, 0) < val:
            self.E[e].wait_ge(self.semo[key], val)
            self.seen[e][key] = val
            self.ninst += 1

    def _deps(self, e, reads, writes):
        for t in reads:
            if t.w is not None:
                self.wait(e, t.w)
        for t in writes:
            if t.w is not None:
                self.wait(e, t.w)
            for key, val in t.r.items():
                self.wait(e, (key, val))

    def _mark(self, tok, reads, writes):
        key, val = tok
        for t in reads:
            t.r[key] = val
        for t in writes:
            t.w = tok
            t.r = {}

    def op(self, e, reads, writes, fn):
        if self.yield_ is not None:
            me = self.stream
            if self.last_e.get(me) not in (None, e):
                self.last_e[me] = e
                self.yield_(me)
                self.stream = me
            self.last_e[me] = e
        self._deps(e, reads, writes)
        ins = fn(self.E[e])
        key = ("c", e)
        self.cnt[key] += 1
        ins.then_inc(self.semo[key], 1)
        self._mark((key, self.cnt[key]), reads, writes)
        self.ninst += 1
        return ins

    def dma(self, q, reads, writes, fn):
        self._deps(q, reads, writes)
        key = self.dq[q].next()
        if self.cnt[key] > 0:
            self.wait(q, (key, self.cnt[key]))
        ins = fn(self.E[q])
        self.cnt[key] += 16
        ins.then_inc(self.semo[key], 16)
        self._mark((key, self.cnt[key]), reads, writes)
        self.ninst += 1
        return ins

    def barrier(self):
        for e in self.E:
            for key, val in self.cnt.items():
                if val > 0 and key != ("c", e):
                    self.wait(e, (key, val))

    def finish(self):
        for key, val in self.cnt.items():
            if val > 0 and key != ("c", "sp"):
                self.wait("sp", (key, val))


class Coop:
    def __init__(self, K):
        self.K = K

    def run(self, fns):
        import threading
        n = len(fns)
        if n == 1:
            fns[0]()
            return
        cv = threading.Condition()
        st = {"turn": 0, "done": [False] * n, "err": None}

        def nxt(me):
            for d in range(1, n + 1):
                c = (me + d) % n
                if not st["done"][c]:
                    return c
            return -1

        def yield_(me):
            with cv:
                t = nxt(me)
                if t == me or t < 0:
                    return
                st["turn"] = t
                cv.notify_all()
                while st["turn"] != me:
                    cv.wait()

        def worker(me):
            with cv:
                while st["turn"] != me:
                    cv.wait()
            try:
                self.K.stream = me
                fns[me]()
            except BaseException as ex:
                st["err"] = ex
            with cv:
                st["done"][me] = True
                st["turn"] = nxt(me)
                cv.notify_all()

        self.K.yield_ = yield_
        self.K.last_e = {}
        ths = [threading.Thread(target=worker, args=(i,)) for i in range(n)]
        for t in ths:
            t.start()
        for t in ths:
            t.join()
        self.K.yield_ = None
        self.K.stream = None
        if st["err"] is not None:
            raise st["err"]


def dram(nc, name, shape, dt, kind):
    return nc.dram_tensor(name, list(shape), dt, kind=kind)


def build(cfg):
    nc = bass.Bass("TRN2", target_bir_lowering=False)
    N, L = cfg.N, cfg.depth
    NT = N // 128
    NST = N // 512
    CAP = cfg.cap
    NSLOT = NE * CAP
    scr = "ExternalOutput" if cfg.debug else "Internal"

    x_in = dram(nc, "x_in", [N, D], F32, "ExternalInput")
    p_in = dram(nc, "p_in", [L, N, PLE], F32, "ExternalInput")
    W = {}
    wshapes = dict(
        emb_ln_g=[1, D], emb_ln_b=[1, D], w_in=[L, D, INW], w_gk_f=[L, 16, 256], b_gk_f=[L, 256],
        w_gk_b=[L, 16, 256], b_gk_b=[L, 256], gla_norm_g=[L, 512], rpb=[L, 8, 15, 31], w_out=[L, D, D],
        ln1_g=[L, D], ln1_b=[L, D], w_router=[L, D, NE], b_router=[L, NE], w_gu=[L, NE, D, 2 * D],
        b_gu=[L, NE, 2 * D], w_down=[L, NE, D, D], b_down=[L, NE, D], w_ple_proj=[L, PLE, D],
        w_ple_gate=[L, D, D], b_ple_gate=[L, D], ln2_g=[L, D], ln2_b=[L, D])
    for k, shp in wshapes.items():
        W[k] = dram(nc, k, shp, F32, "ExternalInput")
    cf_shape, cb_shape = const_shapes(cfg)
    cf_d = dram(nc, "cf", cf_shape, F32, "ExternalInput")
    cb_d = dram(nc, "cb", cb_shape, BF16, "ExternalInput")
    y_out = dram(nc, "y_out", [N, D], F32, "ExternalOutput")

    xres = dram(nc, "xres", [N, D], F32, scr)
    GQK = dram(nc, "gqk", [8, 128, N], BF16, scr)
    VG = dram(nc, "vg", [N, 512], BF16, scr)
    GG = dram(nc, "gg", [N, 512], F32, scr)
    NQ = dram(nc, "nq", [4, 128, N], BF16, scr)
    NK = dram(nc, "nk", [4, 128, N], BF16, scr)
    VN = dram(nc, "vn", [N, 520], BF16, scr)
    RB = dram(nc, "rbk", [NT, 128, 2, 256], BF16, scr)
    SF = dram(nc, "sfk", [NT, 128, 2, 256], BF16, scr)
    OT = dram(nc, "ot", [8, 128, N], BF16, scr)
    RPP = dram(nc, "rpp", [8, 17, 128], F32, scr)
    RECI = dram(nc, "reci", [128, 128, 4], I32, "ExternalInput")
    FSK = dram(nc, "fsk", [8 * (64 * (17 * 128 + 1) + 256)], F32, scr)
    NAMB = dram(nc, "namb", [128, 1024], F32, "ExternalInput")
    NAMI = dram(nc, "nami", [128, 640], F32, "ExternalInput")
    X1 = dram(nc, "x1", [N, D], F32, scr)
    X1B = dram(nc, "x1b", [N + 1, D], BF16, scr)
    REC = dram(nc, "rec", [NSLOT + 128, 4], I32, scr)
    YS = dram(nc, "ys", [4 * N + 128, D], F32, scr)
    GDEC = dram(nc, "gdec", [128, NT * 4], F32, scr)

    with ExitStack() as es:
        K = Kern(nc, es)
        E = K.E
        cf = K.sb("cf_sb", cf_shape, F32)
        cb = K.sb("cb_sb", cb_shape, BF16)
        K.dma("sp", [], [cf], lambda q: q.dma_start(out=cf[:], in_=cf_d.ap()))
        K.dma("sp", [], [cb], lambda q: q.dma_start(out=cb[:], in_=cb_d.ap()))
        CO = const_offsets(cfg)

        def cfs(name):
            o, w = CO["f"][name]
            return cf[:, o:o + w]

        def cbs(name):
            o, w = CO["b"][name]
            return cb[:, o:o + w]

        ident_b = cbs("ident")
        psum_all = [Tk(es.enter_context(nc.psum_tensor(f"ps{i}", [128, 512], F32))) for i in range(8)]
        psum = Ring(psum_all[0:4])
        psum_l = Ring(psum_all[6:8])
        psum_t = Ring(psum_all[4:6])

        def bcast_load(name, src_row_ap, width):
            t = K.sb(name, [128, width], F32)
            K.dma("sp", [], [t], lambda q: q.dma_start(out=t[:], in_=src_row_ap.partition_broadcast(128)))
            return t

        embg = bcast_load("embg", W["emb_ln_g"][0, :], D)
        embb = bcast_load("embb", W["emb_ln_b"][0, :], D)
        K.barrier()

        def layer_norm(xt, g_bc, b_bc, out_t, stats_ring, eng2="pool"):
            st = stats_ring.next()
            for h in range(2):
                K.op("dve", [xt], [st], lambda v, h=h: v.bn_stats(st[:, h * 6:(h + 1) * 6], xt[:, h * 512:(h + 1) * 512]))
            K.op("dve", [st], [st], lambda v: v.bn_aggr(st[:, 12:14], st[:, 0:12]))
            K.op("dve", [st], [st], lambda v: v.tensor_scalar(st[:, 14:15], st[:, 13:14], LN_EPS, None, ALU.add))
            K.op("pool", [st], [st], lambda g: g.tensor_tensor(st[:, 15:16], st[:, 14:15], cfs("neghalf")[:, 0:1], ALU.pow))
            K.op("dve", [xt, st], [out_t], lambda v: v.tensor_scalar(out_t[:], xt[:], st[:, 12:13], st[:, 15:16], ALU.subtract, ALU.mult))
            K.op(eng2, [out_t, g_bc], [out_t], lambda v: v.tensor_tensor(out_t[:], out_t[:], g_bc[:], ALU.mult))
            K.op(eng2, [out_t, b_bc], [out_t], lambda v: v.tensor_tensor(out_t[:], out_t[:], b_bc[:], ALU.add))

        stats_ring = K.ring("lnst", [128, 32], F32, 4)
        YS_tk = Tk(None)
        REC_tk = Tk(None)

        for l in range(L):
            with ExitStack() as esA:
                KA = K
                old_es = K.es
                K.es = esA
                w_in_sb = K.sb("w_in_sb", [128, 8, INW], BF16)
                w_in_v = W["w_in"][l].rearrange("(k p) c -> p k c", p=128)
                for c0 in (0, INW // 2):
                    K.dma("pool", [], [w_in_sb], lambda q, c0=c0: q.dma_start(
                        out=w_in_sb[:, :, c0:c0 + INW // 2], in_=w_in_v[:, :, c0:c0 + INW // 2]))
                wgk = K.sb("wgk", [16, 2, 256], BF16)
                for d_, nm in enumerate(("w_gk_f", "w_gk_b")):
                    K.dma("pool", [], [wgk], lambda q, d_=d_, nm=nm: q.dma_start(out=wgk[:, d_, :], in_=W[nm][l]))
                nbgk = K.sb("nbgk", [128, 4], F32)
                for d_, nm in enumerate(("b_gk_f", "b_gk_b")):
                    K.dma("sp", [], [nbgk], lambda q, d_=d_, nm=nm: q.dma_start(
                        out=nbgk[:, d_ * 2:d_ * 2 + 2], in_=W[nm][l].rearrange("(m p) -> p m", p=128), allow_slow_non_contiguous=True))
                K.op("dve", [nbgk], [nbgk], lambda v: v.tensor_scalar(nbgk[:], nbgk[:], -1.0, None, ALU.mult))
                gdec = K.sb("gdec_sb", [128, NT * 4], F32)

                x_ring = K.ring("xa", [128, D], F32, 8)
                xb_ring = K.ring("xb", [128, D], BF16, 3)
                xT_ring = K.ring("xT", [128, 8, 512], BF16, 2)
                lr_ring = K.ring("lrs", [16, 2, 512], BF16, 2)
                g_ring = K.ring("gt", [128, 512], F32, 6)
                eb_ring = K.ring("eb", [128, 512], F32, 8)
                gl_ring = K.ring("gl", [128, 8, 512], BF16, 2)
                nq_ring = K.ring("nqs", [128, 8, 512], BF16, 2)
                tm_ring = K.ring("tmb", [128, 512], BF16, 3)
                vn_ring = K.ring("vnb", [128, 8, 65], BF16, 3)
                for t_ in vn_ring.items:
                    K.op("pool", [], [t_], lambda g: g.memset(t_[:], 1.0))
                gg_ring = K.ring("ggs", [128, 512], F32, 3)
                segm = cfs("segmask")

                def loadA(st_i):
                    tiles = []
                    for j in range(4):
                        r0 = st_i * 512 + j * 128
                        xt = x_ring.next()
                        srcx = x_in if l == 0 else xres
                        K.dma("sp", [], [xt], lambda q: q.dma_start(out=xt[:], in_=srcx[r0:r0 + 128, :]))
                        tiles.append(xt)
                    return tiles

                def frontA(st_i, xts):
                    t0 = st_i * 512
                    xT = xT_ring.next()
                    for j in range(4):
                        r0 = t0 + j * 128
                        xt = xts[j]
                        if l == 0:
                            layer_norm(xt, embg, embb, xt, stats_ring)
                            K.dma("sp", [xt], [], lambda q: q.dma_start(out=xres[r0:r0 + 128, :], in_=xt[:]))
                        xb = xb_ring.next()
                        K.op("act", [xt], [xb], lambda a: a.copy(xb[:], xt[:]))
                        pt = psum_t.next()
                        ptb = pt.t.ap().bitcast(BF16)
                        for k in range(8):
                            K.op("pe", [xb], [pt], lambda pe, k=k: pe.transpose(ptb[:, k * 128:(k + 1) * 128], xb[:, k * 128:(k + 1) * 128], ident_b))
                        K.op("dve", [pt], [xT], lambda v: v.tensor_copy(xT[:, :, j * 128:(j + 1) * 128], ptb.rearrange("p (k t) -> p k t", k=8)))
                    return xT

                pendA = [loadA(0)]
                if NST > 1:
                    pendA.append(loadA(1))
                xT_next = frontA(0, pendA.pop(0))
                for st_i in range(NST):
                    t0 = st_i * 512
                    if st_i + 2 < NST:
                        pendA.append(loadA(st_i + 2))
                    xT = xT_next
                    if st_i + 1 < NST:
                        xT_next = frontA(st_i + 1, pendA.pop(0))

                    def proj_fm(c0, m, ps_t):
                        for k in range(8):
                            K.op("pe", [xT, w_in_sb], [ps_t], lambda pe, k=k: pe.matmul(
                                ps_t[0:m, :], w_in_sb[:, k, c0:c0 + m], xT[:, k, :], start=(k == 0), stop=(k == 7)))

                    lrs = lr_ring.next()
                    for d_ in range(2):
                        pl = psum.next()
                        proj_fm(C_LRF + 16 * d_, 16, pl)
                        K.op("act", [pl], [lrs], lambda a: a.copy(lrs[:, d_, :], pl[0:16, :]))
                    gl = gl_ring.next()
                    Efac = {}
                    for d_ in range(2):
                        for m in range(2):
                            pu = psum.next()
                            K.op("pe", [lrs, wgk], [pu], lambda pe: pe.matmul(
                                pu[:, :], wgk[:, d_, m * 128:(m + 1) * 128], lrs[:, d_, :], start=True, stop=True))
                            e1 = g_ring.next()
                            col = d_ * 2 + m
                            K.op("act", [pu, nbgk], [e1], lambda a: a.activation(e1[:], pu[:, :], AF.Exp, bias=nbgk[:, col:col + 1], scale=-1.0))
                            K.op("act", [e1], [e1], lambda a: a.activation(e1[:], e1[:], AF.Ln, bias=cfs("one")[:, 0:1], scale=1.0))
                            lg = g_ring.next()
                            K.op("dve", [e1], [lg], lambda v: v.tensor_scalar(lg[:], e1[:], -1.0 / 16.0, None, ALU.mult))
                            bc = g_ring.next()
                            K.op("dve", [lg], [bc], lambda v: v.tensor_tensor_scan(bc[:], segm, lg[:], 0.0, ALU.mult, ALU.add))
                            if d_ == 1:
                                K.op("dve", [lg, bc], [lg], lambda v: v.tensor_tensor(lg[:], lg[:], bc[:], ALU.subtract))
                                for ch in range(4):
                                    K.op("dve", [lg, bc], [lg], lambda v, ch=ch: v.tensor_scalar(
                                        lg[:, ch * 128:(ch + 1) * 128], lg[:, ch * 128:(ch + 1) * 128],
                                        bc[:, ch * 128 + 127:ch * 128 + 128], None, ALU.add))
                                cum = lg
                            else:
                                cum = bc
                            ep = eb_ring.next()
                            en = eb_ring.next()
                            K.op("act", [cum], [ep], lambda a: a.activation(ep[:], cum[:], AF.Exp))
                            K.op("act", [cum], [en], lambda a: a.activation(en[:], cum[:], AF.Exp, scale=-1.0))
                            Efac[(d_, m)] = (ep, en)
                            off = 127 if d_ == 0 else 0
                            ch0 = st_i * 4
                            src = ep.t.ap()[:, off:512:128]
                            dst = gdec.t.ap().rearrange("p (c x) -> p c x", x=4)[:, ch0:ch0 + 4, col]
                            K.op("dve", [ep], [gdec], lambda v: v.tensor_copy(dst, src))
                    for m in range(2):
                        pq = psum.next()
                        proj_fm(C_QG + m * 128, 128, pq)
                        for d_ in range(2):
                            ep = Efac[(d_, m)][0]
                            K.op("dve", [pq, ep], [gl], lambda v, d_=d_: v.scalar_tensor_tensor(
                                gl[:, d_ * 4 + m, :], pq[:, :], 0.125, ep[:], ALU.mult, ALU.mult))
                        pk = psum.next()
                        proj_fm(C_KG + m * 128, 128, pk)
                        for d_ in range(2):
                            en = Efac[(d_, m)][1]
                            K.op("dve", [pk, en], [gl], lambda v, d_=d_: v.tensor_tensor(
                                gl[:, d_ * 4 + 2 + m, :], pk[:, :], en[:], ALU.mult))
                    K.dma("sp", [gl], [], lambda q: q.dma_start(
                        out=GQK.ap()[:, :, t0:t0 + 512].rearrange("a p t -> p a t"), in_=gl[:]))
                    nqs = nq_ring.next()
                    for m in range(4):
                        pq = psum.next()
                        proj_fm(C_QN + m * 128, 128, pq)
                        K.op("act", [pq], [nqs], lambda a: a.activation(nqs[:, m, :], pq[:, :], AF.Copy, scale=0.125))
                        pk = psum.next()
                        proj_fm(C_KN + m * 128, 128, pk)
                        K.op("dve", [pk], [nqs], lambda v: v.tensor_copy(nqs[:, 4 + m, :], pk[:, :]))
                    K.dma("sp", [nqs], [], lambda q: q.dma_start(
                        out=NQ.ap()[:, :, t0:t0 + 512].rearrange("a p t -> p a t"), in_=nqs[:, 0:4, :]))
                    K.dma("sp", [nqs], [], lambda q: q.dma_start(
                        out=NK.ap()[:, :, t0:t0 + 512].rearrange("a p t -> p a t"), in_=nqs[:, 4:8, :]))
                    for j in range(4):
                        r0 = t0 + j * 128
                        tmb = tm_ring.next()
                        vnb = vn_ring.next()
                        ggs = gg_ring.next()
                        for which, c0 in enumerate((C_VG, C_VN, C_GG)):
                            pv = psum.next()
                            for k in range(8):
                                K.op("pe", [xT, w_in_sb], [pv], lambda pe, k=k: pe.matmul(
                                    pv[:, :], xT[:, k, j * 128:(j + 1) * 128], w_in_sb[:, k, c0:c0 + 512],
                                    start=(k == 0), stop=(k == 7)))
                            if which == 0:
                                K.op("act", [pv], [tmb], lambda a: a.copy(tmb[:], pv[:, :]))
                            elif which == 1:
                                K.op("dve", [pv], [vnb], lambda v: v.tensor_copy(vnb[:, :, 0:64], pv.t.ap().rearrange("p (h d) -> p h d", h=8)))
                            else:
                                K.op("act", [pv], [ggs], lambda a: a.copy(ggs[:], pv[:, :]))
                        K.dma("sp", [tmb], [], lambda q: q.dma_start(out=VG[r0:r0 + 128, :], in_=tmb[:]))
                        K.dma("sp", [vnb], [], lambda q: q.dma_start(out=VN[r0:r0 + 128, :], in_=vnb.t.ap().rearrange("p h d -> p (h d)")))
                        K.dma("sp", [ggs], [], lambda q: q.dma_start(out=GG[r0:r0 + 128, :], in_=ggs[:]))
                K.dma("sp", [gdec], [], lambda q: q.dma_start(out=GDEC.ap(), in_=gdec[:]))
                K.barrier()
                K.es = old_es
            if cfg.debug == "A":
                break
            with ExitStack() as esB:
                old_es = K.es
                K.es = esB
                gdec = K.sb("gdecB", [128, NT * 4], F32)
                K.dma("sp", [], [gdec], lambda q: q.dma_start(out=gdec[:], in_=GDEC.ap()))
                normg = bcast_load("normg", W["gla_norm_g"][l, :], 512)
                K.barrier()
                tri_f = cbs("tri_f")
                tri_b = cbs("tri_b")
                kq_ring = K.ring("gk_s", [128, 2, 128], BF16, 8)
                v_ring = K.ring("gv_s", [128, 512], BF16, 8)
                kt_ring = K.ring("ktok", [128, 2, 128], BF16, 4)
                sb_ring = K.ring("gsb", [128, 2, 256], BF16, 6)
                Sst = [K.sb("gS0", [128, 2, 256], F32), K.sb("gS1", [128, 2, 256], F32)]
                tmpS = [K.sb("gtmpS0", [128, 2, 256], F32), K.sb("gtmpS1", [128, 2, 256], F32)]
                for (s0, T) in cfg.seqs:
                    nch = T // 128
                    c0 = s0 // 128

                    def loadS(i):
                        out = []
                        for d_ in range(2):
                            cg = c0 + (i if d_ == 0 else nch - 1 - i)
                            tk0 = cg * 128
                            kq = kq_ring.next()
                            K.dma("sp", [], [kq], lambda q: q.dma_start(out=kq[:], in_=GQK.ap()[d_ * 4 + 2:d_ * 4 + 4, :, tk0:tk0 + 128].rearrange("a p t -> p a t")))
                            vt = v_ring.next()
                            K.dma("sp", [], [vt], lambda q: q.dma_start(out=vt[:], in_=VG[tk0:tk0 + 128, :]))
                            out.append((cg, kq, vt))
                        return out

                    for d_ in range(2):
                        K.op("pool", [], [Sst[d_]], lambda g: g.memset(Sst[d_][:], 0.0))
                    pend = [loadS(0)]
                    if nch > 1:
                        pend.append(loadS(1))
                    for i in range(nch):
                        if i + 2 < nch:
                            pend.append(loadS(i + 2))
                        cur = pend.pop(0)
                        for d_ in range(2):
                            cg, kq, vt = cur[d_]
                            S_, tS = Sst[d_], tmpS[d_]
                            sbt = sb_ring.next()
                            K.op("act", [S_], [sbt], lambda a: a.copy(sbt[:], S_[:]))
                            K.dma("act", [sbt], [], lambda q: q.dma_start(out=(SF if d_ == 0 else RB)[cg], in_=sbt[:]))
                            if i == nch - 1:
                                continue
                            kt = kt_ring.next()
                            ptk = psum.next()
                            ptkb = ptk.t.ap().bitcast(BF16)
                            for m in range(2):
                                K.op("pe", [kq], [ptk], lambda pe: pe.transpose(ptkb[:, m * 128:(m + 1) * 128], kq[:, m, :], ident_b))
                            K.op("act", [ptk], [kt], lambda a: a.copy(kt.t.ap().rearrange("p m t -> p (m t)"), ptkb[:, 0:256]))
                            pu = psum.next()
                            for m in range(2):
                                K.op("pe", [kt, vt], [pu], lambda pe: pe.matmul(pu[:, m * 256:(m + 1) * 256], kt[:, m, :], vt[:, m * 256:(m + 1) * 256], start=True, stop=True))
                            K.op("dve", [pu, S_], [tS], lambda v: v.tensor_tensor(tS.t.ap().rearrange("p m x -> p (m x)"), S_.t.ap().rearrange("p m x -> p (m x)"), pu[:, :], ALU.add))
                            for m in range(2):
                                col = cg * 4 + d_ * 2 + m
                                K.op("dve", [tS, gdec], [S_], lambda v: v.tensor_scalar(S_[:, m, :], tS[:, m, :], gdec[:, col:col + 1], None, ALU.mult))
                K.barrier()
                K.es = old_es
            with ExitStack() as esBC:
                old_es = K.es
                K.es = esBC
                TB = K.sb("na_TB", [128, 8, 1024], BF16)
                TI = K.sb("na_TI", [128, 8, 640], BF16)
                with ExitStack() as esTab:
                    K.es = esTab
                    rpz = K.sb("rpz", [8, 17, 128], F32)
                    K.op("pool", [], [rpz], lambda g: g.memset(rpz[:], 0.0))
                    K.dma("sp", [], [rpz], lambda q: q.dma_start(out=rpz[:, 1:16, 48:79], in_=W["rpb"][l]))
                    K.dma("sp", [rpz], [], lambda q: q.dma_start(out=RPP.ap(), in_=rpz[:]))
                    K.barrier()
                    maskB = K.sb("na_mB", [128, 1024], F32)
                    maskI = K.sb("na_mI", [128, 640], F32)
                    K.dma("sp", [], [maskB], lambda q: q.dma_start(out=maskB[:], in_=NAMB.ap()))
                    K.dma("sp", [], [maskI], lambda q: q.dma_start(out=maskI[:], in_=NAMI.ap()))
                    raw_ring = K.ring("na_raw", [128, 16, 64], F32, 2)
                    Bc = 17 * 128
                    FH = 64 * (Bc + 1) + 256
                    bcr = K.ring("na_bc", [64, 17, 128], F32, 2)
                    for h in range(8):
                        t_ = bcr.next()
                        K.dma("sp", [], [t_], lambda q: q.dma_start(out=t_[:], in_=bass.AP(RPP, h * Bc, [[0, 64], [128, 17], [1, 128]])))
                        K.dma("sp", [t_], [], lambda q: q.dma_start(out=bass.AP(FSK, h * FH, [[Bc + 1, 64], [128, 17], [1, 128]]), in_=t_[:]))
                    K.barrier()
                    for h in range(8):
                        raw = raw_ring.next()
                        for i_ in range(2):
                            src = bass.AP(FSK, h * FH + (1 - i_) * 128 + 63, [[Bc, 64], [128, 16], [1, 64]])
                            K.dma("sp", [], [raw], lambda q: q.dma_start(out=raw[i_ * 64:(i_ + 1) * 64, :, :], in_=src))
                        K.op("dve", [raw, maskB], [TB], lambda v: v.tensor_tensor(TB[:, h, :], raw.t.ap().rearrange("p a w -> p (a w)"), maskB[:], ALU.add))
                        K.op("dve", [raw, maskI], [TI], lambda v: v.tensor_tensor(TI[:, h, :], raw.t.ap()[:, 3:13, :].rearrange("p a w -> p (a w)"), maskI[:], ALU.add))

                    K.barrier()
                    K.es = esBC
                normg = bcast_load("normg2", W["gla_norm_g"][l, :], 512)
                w_out_sb = K.sb("w_out_sb", [128, 8, D], BF16)
                K.dma("pool", [], [w_out_sb], lambda q: q.dma_start(out=w_out_sb[:], in_=W["w_out"][l].rearrange("(k p) c -> p k c", p=128)))
                wr_sb = K.sb("wr_sb", [128, 8, NE], F32)
                K.dma("sp", [], [wr_sb], lambda q: q.dma_start(out=wr_sb[:], in_=W["w_router"][l].rearrange("(k p) c -> p k c", p=128)))
                br_bc = bcast_load("br_bc", W["b_router"][l, :], NE)
                wr_hi = K.sb("wr_hi", [128, 8, NE], BF16)
                wr_lo = K.sb("wr_lo", [128, 8, NE], BF16)
                K.op("act", [wr_sb], [wr_hi], lambda a: a.copy(wr_hi[:], wr_sb[:]))
                K.op("dve", [wr_sb, wr_hi], [wr_lo], lambda v: v.tensor_tensor(wr_lo[:], wr_sb[:], wr_hi[:], ALU.subtract))
                ln1g = bcast_load("ln1g", W["ln1_g"][l, :], D)
                ln1b = bcast_load("ln1b", W["ln1_b"][l, :], D)
                RI = 128
                assert NSLOT % (128 * RI) == 0
                rinit = K.sb("rinit", [128, RI, 4], I32)
                K.dma("sp", [], [rinit], lambda q: q.dma_start(out=rinit[:], in_=RECI.ap()))
                per = 128 * RI
                for s_ in range(0, NSLOT, per):
                    K.dma("sp", [rinit], [], lambda q: q.dma_start(out=REC.ap()[s_:s_ + per, :].rearrange("(p r) f -> p r f", p=128), in_=rinit[:]))
                zrow = K.sb("zrow", [1, D], BF16)
                K.op("pool", [], [zrow], lambda g: g.memset(zrow[:], 0.0))
                K.dma("sp", [zrow], [], lambda q: q.dma_start(out=X1B[N:N + 1, :], in_=zrow[:]))
                mcum = K.sb("mcum", [128, NE], BF16)
                K.op("pool", [], [mcum], lambda g: g.memset(mcum[:], 0.0))
                K.barrier()
                qk_ring = K.ring("gqk_s", [128, 8, 128], BF16, 3)
                v_ring = K.ring("gv_o", [128, 512], BF16, 3)
                gg_ring2 = K.ring("ggl", [128, 512], F32, 3)
                rb_ring = K.ring("rbs", [128, 2, 2, 256], BF16, 3)
                am_ring = K.ring("am", [128, 128], BF16, 6)
                st_ring = K.ring("gst", [128, 16], F32, 3)
                o_ring = K.ring("go", [128, 512], F32, 4)
                ob_ring = K.ring("gob", [128, 512], BF16, 2)
                ot_ring = K.ring("got", [128, 4, 128], BF16, 2)

                def loadO(cg):
                    tk0 = cg * 128
                    qk = qk_ring.next()
                    K.dma("sp", [], [qk], lambda q: q.dma_start(out=qk[:], in_=GQK.ap()[:, :, tk0:tk0 + 128].rearrange("a p t -> p a t")))
                    vt = v_ring.next()
                    K.dma("sp", [], [vt], lambda q: q.dma_start(out=vt[:], in_=VG[tk0:tk0 + 128, :]))
                    ggt = gg_ring2.next()
                    K.dma("sp", [], [ggt], lambda q: q.dma_start(out=ggt[:], in_=GG[tk0:tk0 + 128, :]))
                    rb = rb_ring.next()
                    K.dma("sp", [], [rb], lambda q: q.dma_start(out=rb[:, 0], in_=SF[cg]))
                    K.dma("sp", [], [rb], lambda q: q.dma_start(out=rb[:, 1], in_=RB[cg]))
                    return qk, vt, ggt, rb

                kT_ring = K.ring("na_k", [128, 4, 640], BF16, 2)
                qT_ring = K.ring("na_q", [128, 4, 128], BF16, 3)
                va_ring = K.ring("na_v", [128, 5, 520], BF16, 2)
                pT_ring = K.ring("na_p", [128, 5, 512], BF16, 2)
                on_ring = K.ring("na_o", [128, 512], BF16, 2)
                rs_ring = K.ring("na_rs", [128, 8], F32, 2)
                ont_ring = K.ring("na_ot", [128, 4, 128], BF16, 2)
                pairs = [(s0, T // GRID_W, r) for (s0, T) in cfg.seqs for r in range(0, T // GRID_W, 2)]

                def loadN(pi):
                    s0, rows, r = pairs[pi]
                    if 4 <= r <= rows - 6:
                        B_, nchk, tab, off = r - 4, 5, TI, None
                    else:
                        B_ = 0 if r < 4 else rows - 8
                        nchk, tab, off = 4, TB, B_ - r + 7
                    tq0 = s0 + r * 64
                    tkk = s0 + B_ * 64
                    kT = kT_ring.next()
                    K.dma("sp", [], [kT], lambda q: q.dma_start(out=kT[:, :, 0:nchk * 128], in_=NK.ap()[:, :, tkk:tkk + nchk * 128].rearrange("a p t -> p a t")))
                    qT = qT_ring.next()
                    K.dma("sp", [], [qT], lambda q: q.dma_start(out=qT[:], in_=NQ.ap()[:, :, tq0:tq0 + 128].rearrange("a p t -> p a t")))
                    va = va_ring.next()
                    K.dma("sp", [], [va], lambda q: q.dma_start(out=va[:, 0:nchk, :], in_=VN.ap()[tkk:tkk + nchk * 128, :].rearrange("(j p) c -> p j c", p=128)))
                    return nchk, tab, off, tq0, kT, qT, va

                xc_ring = K.ring("c_x", [128, D], F32, 3)
                h_ring = K.ring("c_h", [128, D], F32, 3)
                x1b_ring = K.ring("c_x1b", [128, D], BF16, 3)
                x1T_ring = K.ring("c_x1T", [128, 2, 8, 128], BF16, 2)
                xhl_ring = K.ring("c_xhl", [128, 2, D], BF16, 2)
                sm_ring = K.ring("c_sm", [128, 8, NE], F32, 3)
                mk_ring = K.ring("c_mk", [128, NE], BF16, 3)
                rc_ring = K.ring("c_rc", [128, 16], I32, 3)
                ident_f = cfs("ident")

                def loadC(ti):
                    r0 = ti * 128
                    xt = xc_ring.next()
                    K.dma("sp", [], [xt], lambda q: q.dma_start(out=xt[:], in_=xres[r0:r0 + 128, :]))
                    return xt

                def bodyO(cg, ld):
                    qk, vt, ggt, rb = ld
                    psum, psum_l = psO, plO
                    tk0 = cg * 128
                    po = psum_l.next()
                    for h in range(4):
                        m, hh = h // 2, h % 2
                        psl = slice(hh * 64, hh * 64 + 64)
                        ams = []
                        for d_ in range(2):
                            pa = psum.next()
                            K.op("pe", [qk], [pa], lambda pe: pe.matmul(pa[:, 0:128], qk[psl, d_ * 4 + 2 + m, :], qk[psl, d_ * 4 + m, :], start=True, stop=True))
                            am = am_ring.next()
                            if d_ == 0:
                                K.op("dve", [pa], [am], lambda v: v.tensor_tensor(am[:], pa[:, 0:128], tri_f, ALU.mult))
                            else:
                                K.op("dve", [pa], [am], lambda v: v.tensor_tensor(am[:], pa[:, 0:128], tri_b, ALU.mult))
                            ams.append(am)
                        osl = po[:, h * 128:(h + 1) * 128]
                        K.op("pe", [ams[0], vt], [po], lambda pe: pe.matmul(osl, ams[0][:], vt[:, h * 128:(h + 1) * 128], start=True, stop=False))
                        K.op("pe", [ams[1], vt], [po], lambda pe: pe.matmul(osl, ams[1][:], vt[:, h * 128:(h + 1) * 128], start=False, stop=False))
                        K.op("pe", [qk, rb], [po], lambda pe: pe.matmul(osl, qk[psl, m, :], rb[psl, 0, m, hh * 128:(hh + 1) * 128], start=False, stop=False))
                        K.op("pe", [qk, rb], [po], lambda pe: pe.matmul(osl, qk[psl, 4 + m, :], rb[psl, 1, m, hh * 128:(hh + 1) * 128], start=False, stop=True))
                    o = o_ring.next()
                    K.op("act", [po], [o], lambda a: a.copy(o[:], po[:, :]))
                    sq = o_ring.next()
                    K.op("pool", [o], [sq], lambda g: g.tensor_tensor(sq[:], o[:], o[:], ALU.mult))
                    stt = st_ring.next()
                    K.op("dve", [sq], [stt], lambda v: v.tensor_reduce(stt[:, 0:4], sq.t.ap().rearrange("p (h d) -> p h d", h=4), AX.X, ALU.add))
                    K.op("dve", [stt], [stt], lambda v: v.tensor_scalar(stt[:, 4:8], stt[:, 0:4], 1.0 / 128.0, RMS_EPS, ALU.mult, ALU.add))
                    K.op("pool", [stt], [stt], lambda g: g.tensor_tensor(stt[:, 8:12], stt[:, 4:8], cfs("neghalf"), ALU.pow))
                    sg = o_ring.next()
                    K.op("act", [ggt], [sg], lambda a: a.activation(sg[:], ggt[:], AF.Silu))
                    K.op("pool", [sg, normg], [sg], lambda g: g.tensor_tensor(sg[:], sg[:], normg[:], ALU.mult))
                    for h in range(4):
                        K.op("dve", [o, stt, sg], [o], lambda v: v.scalar_tensor_tensor(o[:, h * 128:(h + 1) * 128], o[:, h * 128:(h + 1) * 128], stt[:, 8 + h:9 + h], sg[:, h * 128:(h + 1) * 128], ALU.mult, ALU.mult))
                    ob = ob_ring.next()
                    K.op("act", [o], [ob], lambda a: a.copy(ob[:], o[:]))
                    pt = psum.next()
                    ptb = pt.t.ap().bitcast(BF16)
                    for k in range(4):
                        K.op("pe", [ob], [pt], lambda pe: pe.transpose(ptb[:, k * 128:(k + 1) * 128], ob[:, k * 128:(k + 1) * 128], ident_b))
                    ot = ot_ring.next()
                    K.op("act", [pt], [ot], lambda a: a.copy(ot.t.ap().rearrange("p k t -> p (k t)"), ptb[:, 0:512]))
                    return ot

                def bodyN(pi, ld):
                    nchk, tab, off, tq0, kT, qT, va = ld
                    psum, psum_l = psN, plN
                    on = on_ring.next()
                    rs = rs_ring.next()
                    for hg in range(2):
                        pT = pT_ring.next()
                        for j in range(nchk):
                            pst = psum.next()
                            for hh in range(4):
                                h = hg * 4 + hh
                                m = h // 2
                                psl = slice((h % 2) * 64, (h % 2) * 64 + 64)
                                if off is None:
                                    tsl = tab[:, h, j * 128:(j + 1) * 128]
                                else:
                                    tsl = tab[:, h, (off + 2 * j) * 64:(off + 2 * j + 2) * 64]
                                K.op("pe", [kT, qT], [pst], lambda pe: pe.matmul(pst[:, hh * 128:(hh + 1) * 128], kT[psl, m, j * 128:(j + 1) * 128], qT[psl, m, :], start=True, stop=False))
                                K.op("pe", [tab], [pst], lambda pe: pe.matmul(pst[:, hh * 128:(hh + 1) * 128], tsl, ident_b, start=False, stop=True))
                            K.op("act", [pst], [pT], lambda a: a.activation(pT[:, j, :], pst[:, :], AF.Exp))
                        po = psum_l.next()
                        for hh in range(4):
                            h = hg * 4 + hh
                            for j in range(nchk):
                                K.op("pe", [pT, va], [po], lambda pe: pe.matmul(po[:, hh * 65:(hh + 1) * 65], pT[:, j, hh * 128:(hh + 1) * 128], va[:, j, h * 65:(h + 1) * 65], start=(j == 0), stop=(j == nchk - 1)))
                        pov = po.t.ap()[:, 0:260].rearrange("p (h d) -> p h d", h=4)
                        K.op("dve", [po], [rs], lambda v: v.reciprocal(rs[:, hg * 4:(hg + 1) * 4], pov[:, :, 64]))
                        for hh in range(4):
                            h = hg * 4 + hh
                            K.op("dve", [po, rs], [on], lambda v: v.tensor_scalar(on[:, h * 64:(h + 1) * 64], po[:, hh * 65:hh * 65 + 64], rs[:, h:h + 1], None, ALU.mult))
                    pt = psum.next()
                    ptb = pt.t.ap().bitcast(BF16)
                    for k in range(4):
                        K.op("pe", [on], [pt], lambda pe: pe.transpose(ptb[:, k * 128:(k + 1) * 128], on[:, k * 128:(k + 1) * 128], ident_b))
                    ont = ont_ring.next()
                    K.op("act", [pt], [ont], lambda a: a.copy(ont.t.ap().rearrange("p k t -> p (k t)"), ptb[:, 0:512]))
                    return ont

                def stage1C(ti, ot, ont, xt):
                    r0 = ti * 128
                    psum = psC
                    ht = h_ring.next()
                    for n_ in range(2):
                        pm = psum.next()
                        for k in range(8):
                            K.op("pe", [ot, ont, w_out_sb], [pm], lambda pe: pe.matmul(pm[:, :], (ot if k < 4 else ont)[:, k % 4, :], w_out_sb[:, k, n_ * 512:(n_ + 1) * 512], start=(k == 0), stop=(k == 7)))
                        K.op("dve", [pm, xt], [ht], lambda v: v.scalar_tensor_tensor(ht[:, n_ * 512:(n_ + 1) * 512], xt[:, n_ * 512:(n_ + 1) * 512], cfg.alpha, pm[:, :], ALU.mult, ALU.add))
                    layer_norm(ht, ln1g, ln1b, ht, stats_ring)
                    K.dma("sp", [ht], [], lambda q: q.dma_start(out=X1[r0:r0 + 128, :], in_=ht[:]))
                    x1b = x1b_ring.next()
                    K.op("act", [ht], [x1b], lambda a: a.copy(x1b[:], ht[:]))
                    K.dma("sp", [x1b], [], lambda q: q.dma_start(out=X1B[r0:r0 + 128, :], in_=x1b[:]))
                    return ht

                def stage2C(ti, ht):
                    r0 = ti * 128
                    psum = psC
                    xhl = xhl_ring.next()
                    K.op("act", [ht], [xhl], lambda a: a.copy(xhl[:, 0, :], ht[:]))
                    K.op("dve", [ht, xhl], [xhl], lambda v: v.tensor_tensor(xhl[:, 1, :], ht[:], xhl[:, 0, :], ALU.subtract))
                    x1T = x1T_ring.next()
                    for part in range(2):
                        pt = psum.next()
                        ptb = pt.t.ap().bitcast(BF16)
                        for k in range(8):
                            K.op("pe", [xhl], [pt], lambda pe: pe.transpose(ptb[:, k * 128:(k + 1) * 128], xhl[:, part, k * 128:(k + 1) * 128], ident_b))
                        K.op("act", [pt], [x1T], lambda a: a.copy(x1T.t.ap()[:, part].rearrange("p k t -> p (k t)"), ptb[:, :]))
                    plg = psum.next()
                    combos = [(0, wr_hi), (0, wr_lo), (1, wr_hi)]
                    for ci, (part, wt) in enumerate(combos):
                        for k in range(8):
                            K.op("pe", [x1T, wt], [plg], lambda pe: pe.matmul(plg[:, 0:NE], x1T[:, part, k, :], wt[:, k, :], start=(ci == 0 and k == 0), stop=(ci == 2 and k == 7)))
                    sm = sm_ring.next()
                    lgs = sm[:, 0, :]
                    K.op("dve", [plg, br_bc], [sm], lambda v: v.tensor_tensor(lgs, plg[:, 0:NE], br_bc[:], ALU.add))
                    mx8 = sm[:, 1, 0:8]
                    K.op("dve", [sm], [sm], lambda v: v.max(mx8, lgs))
                    mk = mk_ring.next()
                    K.op("dve", [sm], [mk], lambda v: v.tensor_scalar(mk[:], lgs, sm[:, 1, 3:4], None, ALU.is_ge))
                    pps = psum.next()
                    K.op("pe", [mk], [pps], lambda pe: pe.matmul(pps[:, 0:NE], cbs("tri_s"), mk[:], start=True, stop=False))
                    K.op("pe", [mcum], [pps], lambda pe: pe.matmul(pps[:, 0:NE], cbs("ones"), mcum[:], start=False, stop=True))
                    K.op("pool", [mcum, mk], [mcum], lambda g: g.tensor_tensor(mcum[:], mcum[:], mk[:], ALU.add))
                    Dm = sm[:, 2, :]
                    K.op("dve", [pps], [sm], lambda v: v.tensor_tensor(Dm, pps[:, 0:NE], cfs("iotacap"), ALU.add))
                    ovf = sm[:, 3, :]
                    K.op("dve", [pps], [sm], lambda v: v.tensor_scalar(ovf, pps[:, 0:NE], float(CAP), float(NSLOT), ALU.is_ge, ALU.mult))
                    K.op("dve", [sm], [sm], lambda v: v.tensor_tensor(Dm, Dm, ovf, ALU.add))
                    negm = sm[:, 1, 8:9]
                    K.op("dve", [sm], [sm], lambda v: v.tensor_scalar(negm, sm[:, 1, 0:1], -1.0, None, ALU.mult))
                    ex4 = sm[:, 1, 12:16]
                    K.op("act", [sm], [sm], lambda a: a.activation(ex4, sm[:, 1, 0:4], AF.Exp, bias=negm, scale=1.0))
                    zz = sm[:, 1, 9:10]
                    K.op("dve", [sm], [sm], lambda v: v.tensor_reduce(zz, ex4, AX.X, ALU.add))
                    K.op("dve", [sm], [sm], lambda v: v.reciprocal(zz, zz))
                    rc = rc_ring.next()
                    rcf = rc.t.ap().bitcast(F32)
                    destf = sm[:, 1, 16:20]
                    tokf = sm[:, 1, 20:21]
                    K.op("dve", [], [sm], lambda v: v.tensor_scalar(tokf, cfs("pidx"), float(r0), None, ALU.add))
                    oh4 = sm.t.ap()[:, 4:8, :]
                    K.op("dve", [sm], [sm], lambda v: v.tensor_tensor(oh4, bass.AP(sm.t, 0, [[8 * NE, 128], [0, 4], [1, NE]]), bass.AP(sm.t, NE, [[8 * NE, 128], [1, 4], [0, NE]]), ALU.is_equal))
                    K.op("dve", [sm], [sm], lambda v: v.tensor_tensor(oh4, oh4, bass.AP(sm.t, 2 * NE, [[8 * NE, 128], [0, 4], [1, NE]]), ALU.mult))
                    K.op("dve", [sm], [sm], lambda v: v.tensor_reduce(destf, oh4, AX.X, ALU.add))
                    rc4 = rc.t.ap().rearrange("p (k f) -> p k f", f=4)
                    rcf4 = rcf.rearrange("p (k f) -> p k f", f=4)
                    K.op("dve", [sm], [rc], lambda v: v.tensor_copy(rc4[:, :, 0], bass.AP(sm.t, NE + 20, [[8 * NE, 128], [0, 4]])))
                    K.op("dve", [sm], [rc], lambda v: v.tensor_scalar(rcf4[:, :, 1], ex4, zz, None, ALU.mult))
                    K.op("dve", [sm], [rc], lambda v: v.scalar_tensor_tensor(rc4[:, :, 2], bass.AP(sm.t, NE + 20, [[8 * NE, 128], [0, 4]]), 4.0, cfs("k0123"), ALU.mult, ALU.add))
                    isov = sm[:, 1, 24:28]
                    K.op("dve", [sm], [sm], lambda v: v.tensor_scalar(isov, destf, float(NSLOT), None, ALU.is_ge))
                    K.op("dve", [sm], [sm], lambda v: v.tensor_scalar(sm[:, 1, 28:32], isov, -1.0, 1.0, ALU.mult, ALU.add))
                    K.op("dve", [sm], [sm], lambda v: v.tensor_tensor(destf, destf, sm[:, 1, 28:32], ALU.mult))
                    K.op("dve", [sm], [sm], lambda v: v.tensor_scalar(isov, isov, cfs("dummyslot"), None, ALU.mult))
                    K.op("dve", [sm], [sm], lambda v: v.tensor_tensor(destf, destf, isov, ALU.add))
                    K.op("dve", [sm], [rc], lambda v: v.tensor_copy(rc4[:, :, 3], destf))
                    pass
                    for k in range(4):
                        K.dma("pool", [rc], [REC_tk] if cfg.serial_scatter else [], lambda q: q.indirect_dma_start(
                            out=REC.ap()[:, :], out_offset=bass.IndirectOffsetOnAxis(ap=rc[:, 4 * k + 3:4 * k + 4], axis=0),
                            in_=rc[:, 4 * k:4 * k + 4], in_offset=None))

                psO, plO = Ring(psum_all[0:2]), Ring(psum_all[6:7])
                psN, plN = Ring(psum_all[2:4]), Ring(psum_all[7:8])
                psC = Ring(psum_all[4:6])
                coop = Coop(K)
                pO, pN, pC = [loadO(0)], [loadN(0)], [loadC(0)]
                res = {}
                hts = {}
                for i in range(NT + 2):
                    if i + 1 < NT:
                        pO.append(loadO(i + 1))
                        pN.append(loadN(i + 1))
                        pC.append(loadC(i + 1))
                    fns = []
                    if i < NT:
                        ldO, ldN = pO.pop(0), pN.pop(0)
                        fns.append(lambda: res.__setitem__(("o", i), bodyO(i, ldO)))
                        fns.append(lambda: res.__setitem__(("n", i), bodyN(i, ldN)))

                    def cstream():
                        if 1 <= i <= NT:
                            hts[i - 1] = stage1C(i - 1, res.pop(("o", i - 1)), res.pop(("n", i - 1)), pC.pop(0))
                        if 2 <= i <= NT + 1:
                            stage2C(i - 2, hts.pop(i - 2))
                    fns.append(cstream)
                    coop.run(fns)
                K.barrier()
                K.es = old_es
            if cfg.debug == "C":
                break
            with ExitStack() as esD:
                old_es = K.es
                K.es = esD
                wgu_ring = K.ring("d_wgu", [128, 8, 2 * D], BF16, 2)
                wd_ring = K.ring("d_wd", [128, 8, D], BF16, 2)
                bgu_ring = K.ring("d_bgu", [128, 16], F32, 2)
                bd_ring = K.ring("d_bd", [1, D], BF16, 2)
                rec_ring = K.ring("d_rec", [128, 4], I32, 12)
                xg_ring = K.ring("d_xg", [128, D], BF16, 8)
                xsT_ring = K.ring("d_xsT", [128, 8, 512], BF16, 2)
                hT_ring = K.ring("d_hT", [128, 8, 512], BF16, 2)
                t_ring = K.ring("d_t", [128, 512], F32, 6)
                ys_ring = K.ring("d_ys", [128, D], F32, 3)
                GT = CAP // 128

                def load_w(e):
                    wgu = wgu_ring.next()
                    wd = wd_ring.next()
                    bgu = bgu_ring.next()
                    bd = bd_ring.next()
                    K.dma("pool", [], [wgu], lambda q: q.dma_start(out=wgu[:], in_=W["w_gu"][l, e].rearrange("(k p) c -> p k c", p=128)))
                    K.dma("pool", [], [wd], lambda q: q.dma_start(out=wd[:], in_=W["w_down"][l, e].rearrange("(k p) c -> p k c", p=128)))
                    K.dma("sp", [], [bgu], lambda q: q.dma_start(out=bgu[:], in_=W["b_gu"][l, e].rearrange("(m p) -> p m", p=128), allow_slow_non_contiguous=True))
                    K.dma("pool", [], [bd], lambda q: q.dma_start(out=bd[:], in_=W["b_down"][l, e:e + 1, :]))
                    return wgu, wd, bgu, bd

                groups = [(e, g0, min(4, GT - g0)) for e in range(NE) for g0 in range(0, GT, 4)]

                def prep_loads(gi):
                    e, g0, ng = groups[gi]
                    recs, xgs = [], []
                    for j in range(ng):
                        slot0 = e * CAP + (g0 + j) * 128
                        rec = rec_ring.next()
                        K.dma("sp", [], [rec], lambda q: q.dma_start(out=rec[:], in_=REC.ap()[slot0:slot0 + 128, :]))
                        recs.append(rec)
                        xg = xg_ring.next()
                        K.dma("pool", [rec], [xg], lambda q: q.indirect_dma_start(
                            out=xg[:, :], out_offset=None, in_=X1B.ap()[:, :],
                            in_offset=bass.IndirectOffsetOnAxis(ap=rec[:, 0:1], axis=0)))
                        xgs.append(xg)
                    return recs, xgs

                def prep_T(xgs):
                    xsT = xsT_ring.next()
                    for j, xg in enumerate(xgs):
                        pt = psum.next()
                        ptb = pt.t.ap().bitcast(BF16)
                        for k in range(8):
                            K.op("pe", [xg], [pt], lambda pe: pe.transpose(ptb[:, k * 128:(k + 1) * 128], xg[:, k * 128:(k + 1) * 128], ident_b))
                        K.op("act", [pt], [xsT], lambda a: a.copy(xsT[:, :, j * 128:(j + 1) * 128], ptb.rearrange("p (k t) -> p k t", k=8)))
                    return xsT

                nxt = load_w(0)
                recs, xgs = prep_loads(0)
                xsT = prep_T(xgs)
                for gi, (e, g0, ng) in enumerate(groups):
                    if g0 == 0:
                        wgu, wd, bgu, bd = nxt
                        if e + 1 < NE:
                            nxt = load_w(e + 1)
                    W_ = ng * 128
                    hT = hT_ring.next()
                    nrecs = nxgs = None
                    for m in range(8):
                        if m == 4 and gi + 1 < len(groups):
                            nrecs, nxgs = prep_loads(gi + 1)
                        pg = psum.next()
                        for k in range(8):
                            K.op("pe", [xsT, wgu], [pg], lambda pe: pe.matmul(pg[:, 0:W_], wgu[:, k, m * 128:(m + 1) * 128], xsT[:, k, 0:W_], start=(k == 0), stop=(k == 7)))
                        pu = psum.next()
                        for k in range(8):
                            K.op("pe", [xsT, wgu], [pu], lambda pe: pe.matmul(pu[:, 0:W_], wgu[:, k, D + m * 128:D + (m + 1) * 128], xsT[:, k, 0:W_], start=(k == 0), stop=(k == 7)))
                        g1 = t_ring.next()
                        K.op("dve", [pg, bgu], [g1], lambda v: v.tensor_scalar(g1[:, 0:W_], pg[:, 0:W_], bgu[:, m:m + 1], SW_LIM, ALU.add, ALU.min))
                        sg = t_ring.next()
                        K.op("act", [g1], [sg], lambda a: a.activation(sg[:, 0:W_], g1[:, 0:W_], AF.Sigmoid, scale=SW_ALPHA))
                        u1 = t_ring.next()
                        K.op("dve", [pu, bgu], [u1], lambda v: v.tensor_scalar(u1[:, 0:W_], pu[:, 0:W_], bgu[:, 8 + m:9 + m], SW_LIM, ALU.add, ALU.min))
                        K.op("dve", [u1], [u1], lambda v: v.tensor_scalar(u1[:, 0:W_], u1[:, 0:W_], -SW_LIM, 1.0, ALU.max, ALU.add))
                        K.op("pool", [g1, sg], [g1], lambda g: g.tensor_tensor(g1[:, 0:W_], g1[:, 0:W_], sg[:, 0:W_], ALU.mult))
                        K.op("dve", [g1, u1], [hT], lambda v: v.tensor_tensor(hT[:, m, 0:W_], g1[:, 0:W_], u1[:, 0:W_], ALU.mult))
                    nxsT = prep_T(nxgs) if nxgs is not None else None
                    for j in range(ng):
                        rec = recs[j]
                        recf = rec.t.ap().bitcast(F32)
                        ys = ys_ring.next()
                        for n_ in range(2):
                            py = psum.next()
                            for m in range(8):
                                K.op("pe", [hT, wd], [py], lambda pe: pe.matmul(py[:, :], hT[:, m, j * 128:(j + 1) * 128], wd[:, m, n_ * 512:(n_ + 1) * 512], start=(m == 0), stop=False))
                            K.op("pe", [bd], [py], lambda pe: pe.matmul(py[:, :], cb[0:1, CO["b"]["ones"][0]:CO["b"]["ones"][0] + 128], bd[:, n_ * 512:(n_ + 1) * 512], start=False, stop=True))
                            K.op("act", [py, rec], [ys], lambda a: a.activation(ys[:, n_ * 512:(n_ + 1) * 512], py[:, :], AF.Copy, scale=recf[:, 1:2]))
                        K.dma("pool", [ys, rec], [YS_tk] if cfg.serial_scatter else [], lambda q: q.indirect_dma_start(
                            out=YS.ap()[:, :], out_offset=bass.IndirectOffsetOnAxis(ap=rec[:, 2:3], axis=0),
                            in_=ys[:, :], in_offset=None))
                    recs, xgs, xsT = nrecs, nxgs, nxsT
                K.barrier()
                K.es = old_es
            if cfg.debug == "D":
                break
            with ExitStack() as esE:
                old_es = K.es
                K.es = esE
                wpg = K.sb("e_wpg", [128, 8, D], BF16)
                K.dma("pool", [], [wpg], lambda q: q.dma_start(out=wpg[:], in_=W["w_ple_gate"][l].rearrange("(k p) c -> p k c", p=128)))
                wpp = K.sb("e_wpp", [128, 2, D], BF16)
                K.dma("pool", [], [wpp], lambda q: q.dma_start(out=wpp[:], in_=W["w_ple_proj"][l].rearrange("(k p) c -> p k c", p=128)))
                bpg = K.sb("e_bpg", [1, D], BF16)
                K.dma("pool", [], [bpg], lambda q: q.dma_start(out=bpg[:], in_=W["b_ple_gate"][l:l + 1, :]))
                ln2g = bcast_load("ln2g", W["ln2_g"][l, :], D)
                ln2b = bcast_load("ln2b", W["ln2_b"][l, :], D)
                K.barrier()
                x1_ring = K.ring("e_x1", [128, D], F32, 4)
                ys4_ring = K.ring("e_ys4", [128, 4, D], F32, 3)
                p_ring = K.ring("e_p", [128, PLE], F32, 4)
                pb_ring = K.ring("e_pb", [128, PLE], BF16, 3)
                rb_ring2 = K.ring("e_rb", [128, D], BF16, 3)
                rT_ring = K.ring("e_rT", [128, 8, 128], BF16, 3)
                pT_ring2 = K.ring("e_pT", [128, 2, 128], BF16, 3)
                sg_ring = K.ring("e_sg", [128, 512], F32, 3)
                r2_ring = K.ring("e_r2", [128, D], F32, 3)
                dst = xres if l + 1 < L else y_out
                def loadE(ti):
                    r0 = ti * 128
                    x1t = x1_ring.next()
                    K.dma("sp", [], [x1t], lambda q: q.dma_start(out=x1t[:], in_=X1[r0:r0 + 128, :]))
                    ys4 = ys4_ring.next()
                    K.dma("sp", [], [ys4], lambda q: q.dma_start(out=ys4[:], in_=YS.ap()[4 * r0:4 * r0 + 512, :].rearrange("(p k) c -> p k c", k=4)))
                    pt_ = p_ring.next()
                    K.dma("sp", [], [pt_], lambda q: q.dma_start(out=pt_[:], in_=p_in[l, r0:r0 + 128, :]))
                    return x1t, ys4, pt_

                def stage1E(ti, x1t, ys4, pt_):
                    K.op("dve", [x1t, ys4], [x1t], lambda v: v.scalar_tensor_tensor(x1t[:], x1t[:], cfg.alpha, ys4[:, 0, :], ALU.mult, ALU.add))
                    K.op("pool", [x1t, ys4], [x1t], lambda g: g.tensor_tensor(x1t[:], x1t[:], ys4[:, 1, :], ALU.add))
                    K.op("dve", [x1t, ys4], [x1t], lambda v: v.tensor_tensor(x1t[:], x1t[:], ys4[:, 2, :], ALU.add))
                    K.op("pool", [x1t, ys4], [x1t], lambda g: g.tensor_tensor(x1t[:], x1t[:], ys4[:, 3, :], ALU.add))
                    rb_ = rb_ring2.next()
                    K.op("act", [x1t], [rb_], lambda a: a.copy(rb_[:], x1t[:]))
                    pb_ = pb_ring.next()
                    K.op("act", [pt_], [pb_], lambda a: a.copy(pb_[:], pt_[:]))
                    ptr = psum_t.next()
                    ptrb = ptr.t.ap().bitcast(BF16)
                    for k in range(8):
                        K.op("pe", [rb_], [ptr], lambda pe: pe.transpose(ptrb[:, k * 128:(k + 1) * 128], rb_[:, k * 128:(k + 1) * 128], ident_b))
                    rT = rT_ring.next()
                    K.op("dve", [ptr], [rT], lambda v: v.tensor_copy(rT.t.ap().rearrange("p k t -> p (k t)"), ptrb[:, :]))
                    ptp = psum_t.next()
                    ptpb = ptp.t.ap().bitcast(BF16)
                    for k in range(2):
                        K.op("pe", [pb_], [ptp], lambda pe: pe.transpose(ptpb[:, k * 128:(k + 1) * 128], pb_[:, k * 128:(k + 1) * 128], ident_b))
                    pT = pT_ring2.next()
                    K.op("act", [ptp], [pT], lambda a: a.copy(pT.t.ap().rearrange("p k t -> p (k t)"), ptpb[:, 0:256]))
                    return x1t, rT, pT

                pendE = [loadE(0)]
                if NT > 1:
                    pendE.append(loadE(1))
                s1_next = stage1E(0, *pendE.pop(0))
                for ti in range(NT):
                    r0 = ti * 128
                    if ti + 2 < NT:
                        pendE.append(loadE(ti + 2))
                    x1t, rT, pT = s1_next
                    if ti + 1 < NT:
                        s1_next = stage1E(ti + 1, *pendE.pop(0))
                    r2 = r2_ring.next()
                    for n_ in range(2):
                        pgt = psum.next()
                        for k in range(8):
                            K.op("pe", [rT, wpg], [pgt], lambda pe: pe.matmul(pgt[:, :], rT[:, k, :], wpg[:, k, n_ * 512:(n_ + 1) * 512], start=(k == 0), stop=False))
                        K.op("pe", [bpg], [pgt], lambda pe: pe.matmul(pgt[:, :], cb[0:1, CO["b"]["ones"][0]:CO["b"]["ones"][0] + 128], bpg[:, n_ * 512:(n_ + 1) * 512], start=False, stop=True))
                        ppj = psum.next()
                        for k in range(2):
                            K.op("pe", [pT, wpp], [ppj], lambda pe: pe.matmul(ppj[:, :], pT[:, k, :], wpp[:, k, n_ * 512:(n_ + 1) * 512], start=(k == 0), stop=(k == 1)))
                        sg = sg_ring.next()
                        K.op("act", [pgt], [sg], lambda a: a.activation(sg[:], pgt[:, :], AF.Sigmoid))
                        K.op("dve", [sg, ppj], [sg], lambda v: v.tensor_tensor(sg[:], sg[:], ppj[:, :], ALU.mult))
                        K.op("pool", [sg, x1t], [r2], lambda g: g.tensor_tensor(r2[:, n_ * 512:(n_ + 1) * 512], x1t[:, n_ * 512:(n_ + 1) * 512], sg[:], ALU.add))
                    layer_norm(r2, ln2g, ln2b, r2, stats_ring)
                    K.dma("sp", [r2], [], lambda q: q.dma_start(out=dst[r0:r0 + 128, :], in_=r2[:]))
                K.barrier()
                K.es = old_es

        K.barrier()
        K.finish()
    print("instructions:", K.ninst)
    return nc


def const_offsets(cfg):
    f = {}
    o = 0
    for name, w in (("eps_ln", 1), ("one", 1), ("eps_rms", 1), ("segmask", 512), ("ident", 128), ("onesf", 128), ("pidx", 1), ("iotacap", NE), ("dummyslot", 1), ("neghalf", 4), ("k0123", 4)):
        f[name] = (o, w)
        o += w
    fw = o
    b = {}
    o = 0
    for name, w in (("ident", 128), ("ones", 128), ("tri_f", 128), ("tri_b", 128), ("tri_s", 128)):
        b[name] = (o, w)
        o += w
    return {"f": f, "b": b, "fw": fw, "bw": o}


def const_shapes(cfg):
    co = const_offsets(cfg)
    return [128, co["fw"]], [128, co["bw"]]


def make_consts(cfg):
    co = const_offsets(cfg)
    cf = np.zeros((128, co["fw"]), np.float32)
    cbf = np.zeros((128, co["bw"]), np.float32)

    def setf(name, v):
        o, w = co["f"][name]
        cf[:, o:o + w] = v

    def setb(name, v):
        o, w = co["b"][name]
        cbf[:, o:o + w] = v

    setf("eps_ln", LN_EPS)
    setf("one", 1.0)
    setf("eps_rms", RMS_EPS)
    sm = np.ones((128, 512), np.float32)
    sm[:, 0::128] = 0.0
    setf("segmask", sm)
    setf("ident", np.eye(128, dtype=np.float32))
    setf("onesf", 1.0)
    setf("pidx", np.arange(128, dtype=np.float32)[:, None])
    setf("neghalf", -0.5)
    setf("k0123", np.arange(4, dtype=np.float32)[None, :])
    setf("dummyslot", (NE * cfg.cap + np.arange(128, dtype=np.float32))[:, None])
    setf("iotacap", (np.arange(NE, dtype=np.float32) * cfg.cap)[None, :])
    setb("ident", np.eye(128, dtype=np.float32))
    setb("ones", 1.0)
    j = np.arange(128)[:, None]
    i = np.arange(128)[None, :]
    setb("tri_f", (j <= i).astype(np.float32))
    setb("tri_b", (j > i).astype(np.float32))
    setb("tri_s", (j < i).astype(np.float32))
    return cf, cbf.astype(ml_dtypes.bfloat16)


def na_masks():
    p = np.arange(128)
    i_ = (p // 64)[:, None, None]
    c = (p % 64)[:, None, None]
    w = np.arange(64)[None, None, :]
    cs = np.clip(c - 8, 0, 48)
    colok = (w >= cs) & (w < cs + 16)
    mu = np.arange(16)[None, :, None]
    mB = np.where(colok & (mu - i_ >= 0) & (mu - i_ <= 14), 0.0, NEG).astype(np.float32).reshape(128, 1024)
    mm = np.arange(10)[None, :, None]
    mI = np.where(colok & (mm - i_ >= 0) & (mm - i_ <= 7), 0.0, NEG).astype(np.float32).reshape(128, 640)
    return mB, mI


def core_inputs(cfg, inputs, bp, bs, cf, cb):
    m = {}
    m["x_in"] = np.ascontiguousarray(np.concatenate([inputs["x_prompt"][bp], inputs["x_sample"][bs]], axis=0))
    m["p_in"] = np.ascontiguousarray(np.concatenate([inputs["p_prompt"][:cfg.depth, bp], inputs["p_sample"][:cfg.depth, bs]], axis=1))
    for k in ("w_in", "w_gk_f", "b_gk_f", "w_gk_b", "b_gk_b", "gla_norm_g", "rpb", "w_out", "ln1_g", "ln1_b",
              "w_router", "b_router", "w_gu", "b_gu", "w_down", "b_down", "w_ple_proj", "w_ple_gate",
              "b_ple_gate", "ln2_g", "ln2_b"):
        m[k] = np.asarray(inputs[k])[:cfg.depth]
    m["emb_ln_g"] = np.asarray(inputs["emb_ln_g"]).reshape(1, D)
    m["emb_ln_b"] = np.asarray(inputs["emb_ln_b"]).reshape(1, D)
    m["cf"] = cf
    m["cb"] = cb
    m["namb"], m["nami"] = na_masks()
    ri = np.zeros((128, 128, 4), np.int32)
    ri[:, :, 0] = cfg.N
    ri[:, :, 2] = 4 * cfg.N + np.arange(128)[None, :]
    m["reci"] = ri
    return m


def kernel(**inputs):
    cfg = Cfg()
    nc = build(cfg)
    cf, cb = make_consts(cfg)
    n = 8
    in_maps = [core_inputs(cfg, inputs, c, c % 4, cf, cb) for c in range(n)]
    res = run_bass_kernel_spmd(nc, in_maps, core_ids=list(range(n)))
    outs = [np.asarray(r["y_out"]) for r in res.results]
    y_prompt = np.stack([outs[c][:cfg.Tp] for c in range(8)], axis=0).astype(np.float32)
    y_sample = np.stack([outs[c][cfg.Tp:] for c in range(4)], axis=0).astype(np.float32)
    return (y_prompt, y_sample)
```

```python
import numpy as np
import ml_dtypes
from contextlib import ExitStack
import concourse.bass as bass
import concourse.mybir as mybir
from concourse.bass_utils import run_bass_kernel_spmd

F32 = mybir.dt.float32
BF16 = mybir.dt.bfloat16
I32 = mybir.dt.int32
U32 = mybir.dt.uint32
ALU = mybir.AluOpType
AF = mybir.ActivationFunctionType
AX = mybir.AxisListType

D = 1024
PLE = 256
NE = 32
TOPK = 4
GRID_W = 64
INW = 3104
C_QG, C_KG, C_VG, C_GG, C_LRF, C_LRB, C_QN, C_KN, C_VN = 0, 256, 512, 1024, 1536, 1552, 1568, 2080, 2592
SW_ALPHA = 1.702
SW_LIM = 7.0
LN_EPS = 1e-5
RMS_EPS = 1e-6
NEG = -30000.0


class Cfg:
    def __init__(self, Tp=8192, Ts=4096, depth=2, cap=2048, debug=False, serial_scatter=False):
        self.Tp, self.Ts, self.depth, self.cap, self.debug = Tp, Ts, depth, cap, debug
        self.serial_scatter = serial_scatter
        self.N = Tp + Ts
        self.seqs = [(0, Tp), (Tp, Ts)]
        self.alpha = (2 * depth) ** 0.25
        assert Tp % 512 == 0 and Ts % 512 == 0 and cap % 128 == 0


class Tk:
    __slots__ = ("t", "w", "r")

    def __init__(self, t):
        self.t = t
        self.w = None
        self.r = {}

    def __getitem__(self, key):
        return self.t[key]


class Ring:
    def __init__(self, items):
        self.items = items
        self.i = 0

    def next(self):
        it = self.items[self.i % len(self.items)]
        self.i += 1
        return it


class Kern:
    ND = 8

    def __init__(self, nc, es):
        self.nc = nc
        self.es = es
        self.E = {"pe": nc.tensor, "act": nc.scalar, "dve": nc.vector, "pool": nc.gpsimd, "sp": nc.sync}
        self.semo = {}
        self.cnt = {}
        for e in self.E:
            self.semo[("c", e)] = es.enter_context(nc.semaphore("prog_" + e))
            self.cnt[("c", e)] = 0
        self.seen = {e: {} for e in self.E}
        self.dq = {}
        for q in ("sp", "pool", "act"):
            keys = []
            for i in range(self.ND):
                key = ("d", q, i)
                self.semo[key] = es.enter_context(nc.semaphore(f"dma_{q}{i}"))
                self.cnt[key] = 0
                keys.append(key)
            self.dq[q] = Ring(keys)
        self.same_sync = True
        self.ninst = 0
        self.yield_ = None
        self.stream = None
        self.last_e = {}

    def sb(self, name, shape, dt):
        self.uid = getattr(self, "uid", 0) + 1
        return Tk(self.es.enter_context(self.nc.sbuf_tensor(f"{name}_u{self.uid}", shape, dt)))

    def ring(self, name, shape, dt, n):
        return Ring([self.sb(f"{name}{i}", shape, dt) for i in range(n)])

    def wait(self, e, tok):
        key, val = tok
        if key == ("c", e):
            if e == "pe" or e == "sp" or not self.same_sync:
                return
        if self.seen[e].get(key, 0) < val:
            self.E[e].wait_ge(self.semo[key], val)
            self.seen[e][key] = val
            self.ninst += 1

    def _deps(self, e, reads, writes):
        for t in reads:
            if t.w is not None:
                self.wait(e, t.w)
        for t in writes:
            if t.w is not None:
                self.wait(e, t.w)
            for key, val in t.r.items():
                self.wait(e, (key, val))

    def _mark(self, tok, reads, writes):
        key, val = tok
        for t in reads:
            t.r[key] = val
        for t in writes:
            t.w = tok
            t.r = {}

    def op(self, e, reads, writes, fn):
        if self.yield_ is not None:
            me = self.stream
            if self.last_e.get(me) not in (None, e):
                self.last_e[me] = e
                self.yield_(me)
                self.stream = me
            self.last_e[me] = e
        self._deps(e, reads, writes)
        ins = fn(self.E[e])
        key = ("c", e)
        self.cnt[key] += 1
        ins.then_inc(self.semo[key], 1)
        self._mark((key, self.cnt[key]), reads, writes)
        self.ninst += 1
        return ins

    def dma(self, q, reads, writes, fn):
        self._deps(q, reads, writes)
        key = self.dq[q].next()
        if self.cnt[key] > 0:
            self.wait(q, (key, self.cnt[key]))
        ins = fn(self.E[q])
        self.cnt[key] += 16
        ins.then_inc(self.semo[key], 16)
        self._mark((key, self.cnt[key]), reads, writes)
        self.ninst += 1
        return ins

    def barrier(self):
        for e in self.E:
            for key, val in self.cnt.items():
                if val > 0 and key != ("c", e):
                    self.wait(e, (key, val))

    def finish(self):
        for key, val in self.cnt.items():
            if val > 0 and key != ("c", "sp"):
                self.wait("sp", (key, val))


class Coop:
    def __init__(self, K):
        self.K = K

    def run(self, fns):
        import threading
        n = len(fns)
        if n == 1:
            fns[0]()
            return
        cv = threading.Condition()
        st = {"turn": 0, "done": [False] * n, "err": None}

        def nxt(me):
            for d in range(1, n + 1):
                c = (me + d) % n
                if not st["done"][c]:
                    return c
            return -1

        def yield_(me):
            with cv:
                t = nxt(me)
                if t == me or t < 0:
                    return
                st["turn"] = t
                cv.notify_all()
                while st["turn"] != me:
                    cv.wait()

        def worker(me):
            with cv:
                while st["turn"] != me:
                    cv.wait()
            try:
                self.K.stream = me
                fns[me]()
            except BaseException as ex:
                st["err"] = ex
            with cv:
                st["done"][me] = True
                st["turn"] = nxt(me)
                cv.notify_all()

        self.K.yield_ = yield_
        self.K.last_e = {}
        ths = [threading.Thread(target=worker, args=(i,)) for i in range(n)]
        for t in ths:
            t.start()
        for t in ths:
            t.join()
        self.K.yield_ = None
        self.K.stream = None
        if st["err"] is not None:
            raise st["err"]


def dram(nc, name, shape, dt, kind):
    return nc.dram_tensor(name, list(shape), dt, kind=kind)


def build(cfg):
    nc = bass.Bass("TRN2", target_bir_lowering=False)
    N, L = cfg.N, cfg.depth
    NT = N // 128
    NST = N // 512
    CAP = cfg.cap
    NSLOT = NE * CAP
    scr = "ExternalOutput" if cfg.debug else "Internal"

    x_in = dram(nc, "x_in", [N, D], F32, "ExternalInput")
    p_in = dram(nc, "p_in", [L, N, PLE], F32, "ExternalInput")
    W = {}
    wshapes = dict(
        emb_ln_g=[1, D], emb_ln_b=[1, D], w_in=[L, D, INW], w_gk_f=[L, 16, 256], b_gk_f=[L, 256],
        w_gk_b=[L, 16, 256], b_gk_b=[L, 256], gla_norm_g=[L, 512], rpb=[L, 8, 15, 31], w_out=[L, D, D],
        ln1_g=[L, D], ln1_b=[L, D], w_router=[L, D, NE], b_router=[L, NE], w_gu=[L, NE, D, 2 * D],
        b_gu=[L, NE, 2 * D], w_down=[L, NE, D, D], b_down=[L, NE, D], w_ple_proj=[L, PLE, D],
        w_ple_gate=[L, D, D], b_ple_gate=[L, D], ln2_g=[L, D], ln2_b=[L, D])
    for k, shp in wshapes.items():
        W[k] = dram(nc, k, shp, F32, "ExternalInput")
    cf_shape, cb_shape = const_shapes(cfg)
    cf_d = dram(nc, "cf", cf_shape, F32, "ExternalInput")
    cb_d = dram(nc, "cb", cb_shape, BF16, "ExternalInput")
    y_out = dram(nc, "y_out", [N, D], F32, "ExternalOutput")

    xres = dram(nc, "xres", [N, D], F32, scr)
    GQK = dram(nc, "gqk", [8, 128, N], BF16, scr)
    VG = dram(nc, "vg", [N, 512], BF16, scr)
    GG = dram(nc, "gg", [N, 512], F32, scr)
    NQ = dram(nc, "nq", [4, 128, N], BF16, scr)
    NK = dram(nc, "nk", [4, 128, N], BF16, scr)
    VN = dram(nc, "vn", [N, 520], BF16, scr)
    RB = dram(nc, "rbk", [NT, 128, 2, 256], BF16, scr)
    SF = dram(nc, "sfk", [NT, 128, 2, 256], BF16, scr)
    OT = dram(nc, "ot", [8, 128, N], BF16, scr)
    RPP = dram(nc, "rpp", [8, 17, 128], F32, scr)
    RECI = dram(nc, "reci", [128, 128, 4], I32, "ExternalInput")
    FSK = dram(nc, "fsk", [8 * (64 * (17 * 128 + 1) + 256)], F32, scr)
    NAMB = dram(nc, "namb", [128, 1024], F32, "ExternalInput")
    NAMI = dram(nc, "nami", [128, 640], F32, "ExternalInput")
    X1 = dram(nc, "x1", [N, D], F32, scr)
    X1B = dram(nc, "x1b", [N + 1, D], BF16, scr)
    REC = dram(nc, "rec", [NSLOT + 128, 4], I32, scr)
    YS = dram(nc, "ys", [4 * N + 128, D], F32, scr)
    GDEC = dram(nc, "gdec", [128, NT * 4], F32, scr)

    with ExitStack() as es:
        K = Kern(nc, es)
        E = K.E
        cf = K.sb("cf_sb", cf_shape, F32)
        cb = K.sb("cb_sb", cb_shape, BF16)
        K.dma("sp", [], [cf], lambda q: q.dma_start(out=cf[:], in_=cf_d.ap()))
        K.dma("sp", [], [cb], lambda q: q.dma_start(out=cb[:], in_=cb_d.ap()))
        CO = const_offsets(cfg)

        def cfs(name):
            o, w = CO["f"][name]
            return cf[:, o:o + w]

        def cbs(name):
            o, w = CO["b"][name]
            return cb[:, o:o + w]

        ident_b = cbs("ident")
        psum_all = [Tk(es.enter_context(nc.psum_tensor(f"ps{i}", [128, 512], F32))) for i in range(8)]
        psum = Ring(psum_all[0:4])
        psum_l = Ring(psum_all[6:8])
        psum_t = Ring(psum_all[4:6])

        def bcast_load(name, src_row_ap, width):
            t = K.sb(name, [128, width], F32)
            K.dma("sp", [], [t], lambda q: q.dma_start(out=t[:], in_=src_row_ap.partition_broadcast(128)))
            return t

        embg = bcast_load("embg", W["emb_ln_g"][0, :], D)
        embb = bcast_load("embb", W["emb_ln_b"][0, :], D)
        K.barrier()

        def layer_norm(xt, g_bc, b_bc, out_t, stats_ring, eng2="pool"):
            st = stats_ring.next()
            for h in range(2):
                K.op("dve", [xt], [st], lambda v, h=h: v.bn_stats(st[:, h * 6:(h + 1) * 6], xt[:, h * 512:(h + 1) * 512]))
            K.op("dve", [st], [st], lambda v: v.bn_aggr(st[:, 12:14], st[:, 0:12]))
            K.op("dve", [st], [st], lambda v: v.tensor_scalar(st[:, 14:15], st[:, 13:14], LN_EPS, None, ALU.add))
            K.op("pool", [st], [st], lambda g: g.tensor_tensor(st[:, 15:16], st[:, 14:15], cfs("neghalf")[:, 0:1], ALU.pow))
            K.op("dve", [xt, st], [out_t], lambda v: v.tensor_scalar(out_t[:], xt[:], st[:, 12:13], st[:, 15:16], ALU.subtract, ALU.mult))
            K.op(eng2, [out_t, g_bc], [out_t], lambda v: v.tensor_tensor(out_t[:], out_t[:], g_bc[:], ALU.mult))
            K.op(eng2, [out_t, b_bc], [out_t], lambda v: v.tensor_tensor(out_t[:], out_t[:], b_bc[:], ALU.add))

        stats_ring = K.ring("lnst", [128, 32], F32, 6)
        YS_tk = Tk(None)
        REC_tk = Tk(None)

        for l in range(L):
            with ExitStack() as esA:
                KA = K
                old_es = K.es
                K.es = esA
                w_in_sb = K.sb("w_in_sb", [128, 8, INW], BF16)
                w_in_v = W["w_in"][l].rearrange("(k p) c -> p k c", p=128)
                for c0 in (0, INW // 2):
                    K.dma("pool", [], [w_in_sb], lambda q, c0=c0: q.dma_start(
                        out=w_in_sb[:, :, c0:c0 + INW // 2], in_=w_in_v[:, :, c0:c0 + INW // 2]))
                wgk = K.sb("wgk", [16, 2, 256], BF16)
                for d_, nm in enumerate(("w_gk_f", "w_gk_b")):
                    K.dma("pool", [], [wgk], lambda q, d_=d_, nm=nm: q.dma_start(out=wgk[:, d_, :], in_=W[nm][l]))
                nbgk = K.sb("nbgk", [128, 4], F32)
                for d_, nm in enumerate(("b_gk_f", "b_gk_b")):
                    K.dma("sp", [], [nbgk], lambda q, d_=d_, nm=nm: q.dma_start(
                        out=nbgk[:, d_ * 2:d_ * 2 + 2], in_=W[nm][l].rearrange("(m p) -> p m", p=128), allow_slow_non_contiguous=True))
                K.op("dve", [nbgk], [nbgk], lambda v: v.tensor_scalar(nbgk[:], nbgk[:], -1.0, None, ALU.mult))
                gdec = K.sb("gdec_sb", [128, NT * 4], F32)

                x_ring = K.ring("xa", [128, D], F32, 8)
                xb_ring = K.ring("xb", [128, D], BF16, 3)
                xT_ring = K.ring("xT", [128, 8, 512], BF16, 2)
                lr_ring = K.ring("lrs", [16, 2, 512], BF16, 2)
                g_ring = K.ring("gt", [128, 512], F32, 6)
                eb_ring = K.ring("eb", [128, 512], F32, 8)
                gl_ring = K.ring("gl", [128, 8, 512], BF16, 2)
                nq_ring = K.ring("nqs", [128, 8, 512], BF16, 2)
                tm_ring = K.ring("tmb", [128, 512], BF16, 3)
                vn_ring = K.ring("vnb", [128, 8, 65], BF16, 3)
                for t_ in vn_ring.items:
                    K.op("pool", [], [t_], lambda g: g.memset(t_[:], 1.0))
                gg_ring = K.ring("ggs", [128, 512], F32, 3)
                segm = cfs("segmask")

                def loadA(st_i):
                    tiles = []
                    for j in range(4):
                        r0 = st_i * 512 + j * 128
                        xt = x_ring.next()
                        srcx = x_in if l == 0 else xres
                        K.dma("sp", [], [xt], lambda q: q.dma_start(out=xt[:], in_=srcx[r0:r0 + 128, :]))
                        tiles.append(xt)
                    return tiles

                def frontA(st_i, xts):
                    t0 = st_i * 512
                    xT = xT_ring.next()
                    for j in range(4):
                        r0 = t0 + j * 128
                        xt = xts[j]
                        if l == 0:
                            layer_norm(xt, embg, embb, xt, stats_ring)
                            K.dma("sp", [xt], [], lambda q: q.dma_start(out=xres[r0:r0 + 128, :], in_=xt[:]))
                        xb = xb_ring.next()
                        K.op("act", [xt], [xb], lambda a: a.copy(xb[:], xt[:]))
                        pt = psum_t.next()
                        ptb = pt.t.ap().bitcast(BF16)
                        for k in range(8):
                            K.op("pe", [xb], [pt], lambda pe, k=k: pe.transpose(ptb[:, k * 128:(k + 1) * 128], xb[:, k * 128:(k + 1) * 128], ident_b))
                        K.op("dve", [pt], [xT], lambda v: v.tensor_copy(xT[:, :, j * 128:(j + 1) * 128], ptb.rearrange("p (k t) -> p k t", k=8)))
                    return xT

                psA1, psA2, psA3 = Ring(psum_all[0:2]), Ring(psum_all[2:4]), Ring(psum_all[6:8])
                coopA = Coop(K)
                pendA = [loadA(0)]
                if NST > 1:
                    pendA.append(loadA(1))
                xT_next = frontA(0, pendA.pop(0))
                for st_i in range(NST):
                    t0 = st_i * 512
                    if st_i + 2 < NST:
                        pendA.append(loadA(st_i + 2))
                    xT = xT_next
                    nxt_box = {}

                    def proj_fm(c0, m, ps_t):
                        for k in range(8):
                            K.op("pe", [xT, w_in_sb], [ps_t], lambda pe, k=k: pe.matmul(
                                ps_t[0:m, :], w_in_sb[:, k, c0:c0 + m], xT[:, k, :], start=(k == 0), stop=(k == 7)))

                    def backS1():
                        psum = psA1
                        lrs = lr_ring.next()
                        for d_ in range(2):
                            pl = psum.next()
                            proj_fm(C_LRF + 16 * d_, 16, pl)
                            K.op("act", [pl], [lrs], lambda a: a.copy(lrs[:, d_, :], pl[0:16, :]))
                        gl = gl_ring.next()
                        Efac = {}
                        for d_ in range(2):
                            for m in range(2):
                                pu = psum.next()
                                K.op("pe", [lrs, wgk], [pu], lambda pe: pe.matmul(
                                    pu[:, :], wgk[:, d_, m * 128:(m + 1) * 128], lrs[:, d_, :], start=True, stop=True))
                                e1 = g_ring.next()
                                col = d_ * 2 + m
                                K.op("act", [pu, nbgk], [e1], lambda a: a.activation(e1[:], pu[:, :], AF.Exp, bias=nbgk[:, col:col + 1], scale=-1.0))
                                K.op("act", [e1], [e1], lambda a: a.activation(e1[:], e1[:], AF.Ln, bias=cfs("one")[:, 0:1], scale=1.0))
                                lg = g_ring.next()
                                K.op("dve", [e1], [lg], lambda v: v.tensor_scalar(lg[:], e1[:], -1.0 / 16.0, None, ALU.mult))
                                bc = g_ring.next()
                                K.op("dve", [lg], [bc], lambda v: v.tensor_tensor_scan(bc[:], segm, lg[:], 0.0, ALU.mult, ALU.add))
                                if d_ == 1:
                                    K.op("dve", [lg, bc], [lg], lambda v: v.tensor_tensor(lg[:], lg[:], bc[:], ALU.subtract))
                                    for ch in range(4):
                                        K.op("dve", [lg, bc], [lg], lambda v, ch=ch: v.tensor_scalar(
                                            lg[:, ch * 128:(ch + 1) * 128], lg[:, ch * 128:(ch + 1) * 128],
                                            bc[:, ch * 128 + 127:ch * 128 + 128], None, ALU.add))
                                    cum = lg
                                else:
                                    cum = bc
                                ep = eb_ring.next()
                                en = eb_ring.next()
                                K.op("act", [cum], [ep], lambda a: a.activation(ep[:], cum[:], AF.Exp))
                                K.op("act", [cum], [en], lambda a: a.activation(en[:], cum[:], AF.Exp, scale=-1.0))
                                Efac[(d_, m)] = (ep, en)
                                off = 127 if d_ == 0 else 0
                                ch0 = st_i * 4
                                src = ep.t.ap()[:, off:512:128]
                                dst = gdec.t.ap().rearrange("p (c x) -> p c x", x=4)[:, ch0:ch0 + 4, col]
                                K.op("dve", [ep], [gdec], lambda v: v.tensor_copy(dst, src))
                        for m in range(2):
                            pq = psum.next()
                            proj_fm(C_QG + m * 128, 128, pq)
                            for d_ in range(2):
                                ep = Efac[(d_, m)][0]
                                K.op("dve", [pq, ep], [gl], lambda v, d_=d_: v.scalar_tensor_tensor(
                                    gl[:, d_ * 4 + m, :], pq[:, :], 0.125, ep[:], ALU.mult, ALU.mult))
                            pk = psum.next()
                            proj_fm(C_KG + m * 128, 128, pk)
                            for d_ in range(2):
                                en = Efac[(d_, m)][1]
                                K.op("dve", [pk, en], [gl], lambda v, d_=d_: v.tensor_tensor(
                                    gl[:, d_ * 4 + 2 + m, :], pk[:, :], en[:], ALU.mult))
                        K.dma("sp", [gl], [], lambda q: q.dma_start(
                            out=GQK.ap()[:, :, t0:t0 + 512].rearrange("a p t -> p a t"), in_=gl[:]))

                    def backS2():
                        psum = psA2
                        nqs = nq_ring.next()
                        for m in range(4):
                            pq = psum.next()
                            proj_fm(C_QN + m * 128, 128, pq)
                            K.op("act", [pq], [nqs], lambda a: a.activation(nqs[:, m, :], pq[:, :], AF.Copy, scale=0.125))
                            pk = psum.next()
                            proj_fm(C_KN + m * 128, 128, pk)
                            K.op("dve", [pk], [nqs], lambda v: v.tensor_copy(nqs[:, 4 + m, :], pk[:, :]))
                        K.dma("sp", [nqs], [], lambda q: q.dma_start(
                            out=NQ.ap()[:, :, t0:t0 + 512].rearrange("a p t -> p a t"), in_=nqs[:, 0:4, :]))
                        K.dma("sp", [nqs], [], lambda q: q.dma_start(
                            out=NK.ap()[:, :, t0:t0 + 512].rearrange("a p t -> p a t"), in_=nqs[:, 4:8, :]))

                    def backS3():
                        psum = psA3
                        for j in range(4):
                            r0 = t0 + j * 128
                            tmb = tm_ring.next()
                            vnb = vn_ring.next()
                            ggs = gg_ring.next()
                            for which, c0 in enumerate((C_VG, C_VN, C_GG)):
                                pv = psum.next()
                                for k in range(8):
                                    K.op("pe", [xT, w_in_sb], [pv], lambda pe, k=k: pe.matmul(
                                        pv[:, :], xT[:, k, j * 128:(j + 1) * 128], w_in_sb[:, k, c0:c0 + 512],
                                        start=(k == 0), stop=(k == 7)))
                                if which == 0:
                                    K.op("act", [pv], [tmb], lambda a: a.copy(tmb[:], pv[:, :]))
                                elif which == 1:
                                    K.op("dve", [pv], [vnb], lambda v: v.tensor_copy(vnb[:, :, 0:64], pv.t.ap().rearrange("p (h d) -> p h d", h=8)))
                                else:
                                    K.op("act", [pv], [ggs], lambda a: a.copy(ggs[:], pv[:, :]))
                            K.dma("sp", [tmb], [], lambda q: q.dma_start(out=VG[r0:r0 + 128, :], in_=tmb[:]))
                            K.dma("sp", [vnb], [], lambda q: q.dma_start(out=VN[r0:r0 + 128, :], in_=vnb.t.ap().rearrange("p h d -> p (h d)")))
                            K.dma("sp", [ggs], [], lambda q: q.dma_start(out=GG[r0:r0 + 128, :], in_=ggs[:]))

                    fnsA = [backS1, backS2, backS3]
                    if st_i + 1 < NST:
                        ldn = pendA.pop(0)
                        fnsA.append(lambda: nxt_box.__setitem__("xT", frontA(st_i + 1, ldn)))
                    coopA.run(fnsA)
                    xT_next = nxt_box.get("xT")
                K.dma("sp", [gdec], [], lambda q: q.dma_start(out=GDEC.ap(), in_=gdec[:]))
                K.barrier()
                K.es = old_es
            if cfg.debug == "A":
                break
            with ExitStack() as esB:
                old_es = K.es
                K.es = esB
                gdec = K.sb("gdecB", [128, NT * 4], F32)
                K.dma("sp", [], [gdec], lambda q: q.dma_start(out=gdec[:], in_=GDEC.ap()))
                normg = bcast_load("normg", W["gla_norm_g"][l, :], 512)
                K.barrier()
                tri_f = cbs("tri_f")
                tri_b = cbs("tri_b")
                kq_ring = K.ring("gk_s", [128, 2, 128], BF16, 8)
                v_ring = K.ring("gv_s", [128, 512], BF16, 8)
                kt_ring = K.ring("ktok", [128, 2, 128], BF16, 4)
                sb_ring = K.ring("gsb", [128, 2, 256], BF16, 6)
                Sst = [K.sb("gS0", [128, 2, 256], F32), K.sb("gS1", [128, 2, 256], F32)]
                tmpS = [K.sb("gtmpS0", [128, 2, 256], F32), K.sb("gtmpS1", [128, 2, 256], F32)]
                for (s0, T) in cfg.seqs:
                    nch = T // 128
                    c0 = s0 // 128

                    def loadS(i):
                        out = []
                        for d_ in range(2):
                            cg = c0 + (i if d_ == 0 else nch - 1 - i)
                            tk0 = cg * 128
                            kq = kq_ring.next()
                            K.dma("sp", [], [kq], lambda q: q.dma_start(out=kq[:], in_=GQK.ap()[d_ * 4 + 2:d_ * 4 + 4, :, tk0:tk0 + 128].rearrange("a p t -> p a t")))
                            vt = v_ring.next()
                            K.dma("sp", [], [vt], lambda q: q.dma_start(out=vt[:], in_=VG[tk0:tk0 + 128, :]))
                            out.append((cg, kq, vt))
                        return out

                    for d_ in range(2):
                        K.op("pool", [], [Sst[d_]], lambda g: g.memset(Sst[d_][:], 0.0))
                    pend = [loadS(0)]
                    if nch > 1:
                        pend.append(loadS(1))
                    for i in range(nch):
                        if i + 2 < nch:
                            pend.append(loadS(i + 2))
                        cur = pend.pop(0)
                        for d_ in range(2):
                            cg, kq, vt = cur[d_]
                            S_, tS = Sst[d_], tmpS[d_]
                            sbt = sb_ring.next()
                            K.op("act", [S_], [sbt], lambda a: a.copy(sbt[:], S_[:]))
                            K.dma("act", [sbt], [], lambda q: q.dma_start(out=(SF if d_ == 0 else RB)[cg], in_=sbt[:]))
                            if i == nch - 1:
                                continue
                            kt = kt_ring.next()
                            ptk = psum.next()
                            ptkb = ptk.t.ap().bitcast(BF16)
                            for m in range(2):
                                K.op("pe", [kq], [ptk], lambda pe: pe.transpose(ptkb[:, m * 128:(m + 1) * 128], kq[:, m, :], ident_b))
                            K.op("act", [ptk], [kt], lambda a: a.copy(kt.t.ap().rearrange("p m t -> p (m t)"), ptkb[:, 0:256]))
                            pu = psum.next()
                            for m in range(2):
                                K.op("pe", [kt, vt], [pu], lambda pe: pe.matmul(pu[:, m * 256:(m + 1) * 256], kt[:, m, :], vt[:, m * 256:(m + 1) * 256], start=True, stop=True))
                            K.op("dve", [pu, S_], [tS], lambda v: v.tensor_tensor(tS.t.ap().rearrange("p m x -> p (m x)"), S_.t.ap().rearrange("p m x -> p (m x)"), pu[:, :], ALU.add))
                            for m in range(2):
                                col = cg * 4 + d_ * 2 + m
                                K.op("dve", [tS, gdec], [S_], lambda v: v.tensor_scalar(S_[:, m, :], tS[:, m, :], gdec[:, col:col + 1], None, ALU.mult))
                K.barrier()
                K.es = old_es
            with ExitStack() as esBC:
                old_es = K.es
                K.es = esBC
                TB = K.sb("na_TB", [128, 8, 1024], BF16)
                TI = K.sb("na_TI", [128, 8, 640], BF16)
                with ExitStack() as esTab:
                    K.es = esTab
                    rpz = K.sb("rpz", [8, 17, 128], F32)
                    K.op("pool", [], [rpz], lambda g: g.memset(rpz[:], 0.0))
                    K.dma("sp", [], [rpz], lambda q: q.dma_start(out=rpz[:, 1:16, 48:79], in_=W["rpb"][l]))
                    K.dma("sp", [rpz], [], lambda q: q.dma_start(out=RPP.ap(), in_=rpz[:]))
                    K.barrier()
                    maskB = K.sb("na_mB", [128, 1024], F32)
                    maskI = K.sb("na_mI", [128, 640], F32)
                    K.dma("sp", [], [maskB], lambda q: q.dma_start(out=maskB[:], in_=NAMB.ap()))
                    K.dma("sp", [], [maskI], lambda q: q.dma_start(out=maskI[:], in_=NAMI.ap()))
                    raw_ring = K.ring("na_raw", [128, 16, 64], F32, 2)
                    Bc = 17 * 128
                    FH = 64 * (Bc + 1) + 256
                    bcr = K.ring("na_bc", [64, 17, 128], F32, 2)
                    for h in range(8):
                        t_ = bcr.next()
                        K.dma("sp", [], [t_], lambda q: q.dma_start(out=t_[:], in_=bass.AP(RPP, h * Bc, [[0, 64], [128, 17], [1, 128]])))
                        K.dma("sp", [t_], [], lambda q: q.dma_start(out=bass.AP(FSK, h * FH, [[Bc + 1, 64], [128, 17], [1, 128]]), in_=t_[:]))
                    K.barrier()
                    for h in range(8):
                        raw = raw_ring.next()
                        for i_ in range(2):
                            src = bass.AP(FSK, h * FH + (1 - i_) * 128 + 63, [[Bc, 64], [128, 16], [1, 64]])
                            K.dma("sp", [], [raw], lambda q: q.dma_start(out=raw[i_ * 64:(i_ + 1) * 64, :, :], in_=src))
                        K.op("dve", [raw, maskB], [TB], lambda v: v.tensor_tensor(TB[:, h, :], raw.t.ap().rearrange("p a w -> p (a w)"), maskB[:], ALU.add))
                        K.op("dve", [raw, maskI], [TI], lambda v: v.tensor_tensor(TI[:, h, :], raw.t.ap()[:, 3:13, :].rearrange("p a w -> p (a w)"), maskI[:], ALU.add))

                    K.barrier()
                    K.es = esBC
                normg = bcast_load("normg2", W["gla_norm_g"][l, :], 512)
                w_out_sb = K.sb("w_out_sb", [128, 8, D], BF16)
                K.dma("pool", [], [w_out_sb], lambda q: q.dma_start(out=w_out_sb[:], in_=W["w_out"][l].rearrange("(k p) c -> p k c", p=128)))
                wr_sb = K.sb("wr_sb", [128, 8, NE], F32)
                K.dma("sp", [], [wr_sb], lambda q: q.dma_start(out=wr_sb[:], in_=W["w_router"][l].rearrange("(k p) c -> p k c", p=128)))
                br_bc = bcast_load("br_bc", W["b_router"][l, :], NE)
                wr_hi = K.sb("wr_hi", [128, 8, NE], BF16)
                wr_lo = K.sb("wr_lo", [128, 8, NE], BF16)
                K.op("act", [wr_sb], [wr_hi], lambda a: a.copy(wr_hi[:], wr_sb[:]))
                K.op("dve", [wr_sb, wr_hi], [wr_lo], lambda v: v.tensor_tensor(wr_lo[:], wr_sb[:], wr_hi[:], ALU.subtract))
                ln1g = bcast_load("ln1g", W["ln1_g"][l, :], D)
                ln1b = bcast_load("ln1b", W["ln1_b"][l, :], D)
                RI = 128
                assert NSLOT % (128 * RI) == 0
                rinit = K.sb("rinit", [128, RI, 4], I32)
                K.dma("sp", [], [rinit], lambda q: q.dma_start(out=rinit[:], in_=RECI.ap()))
                per = 128 * RI
                for s_ in range(0, NSLOT, per):
                    K.dma("sp", [rinit], [], lambda q: q.dma_start(out=REC.ap()[s_:s_ + per, :].rearrange("(p r) f -> p r f", p=128), in_=rinit[:]))
                zrow = K.sb("zrow", [1, D], BF16)
                K.op("pool", [], [zrow], lambda g: g.memset(zrow[:], 0.0))
                K.dma("sp", [zrow], [], lambda q: q.dma_start(out=X1B[N:N + 1, :], in_=zrow[:]))
                mcum = K.sb("mcum", [128, NE], BF16)
                K.op("pool", [], [mcum], lambda g: g.memset(mcum[:], 0.0))
                K.barrier()
                qk_ring = K.ring("gqk_s", [128, 8, 128], BF16, 3)
                v_ring = K.ring("gv_o", [128, 512], BF16, 3)
                gg_ring2 = K.ring("ggl", [128, 512], F32, 3)
                rb_ring = K.ring("rbs", [128, 2, 2, 256], BF16, 3)
                am_ring = K.ring("am", [128, 128], BF16, 6)
                st_ring = K.ring("gst", [128, 16], F32, 3)
                o_ring = K.ring("go", [128, 512], F32, 4)
                ob_ring = K.ring("gob", [128, 512], BF16, 2)
                ot_ring = K.ring("got", [128, 4, 128], BF16, 2)

                def loadO(cg):
                    tk0 = cg * 128
                    qk = qk_ring.next()
                    K.dma("sp", [], [qk], lambda q: q.dma_start(out=qk[:], in_=GQK.ap()[:, :, tk0:tk0 + 128].rearrange("a p t -> p a t")))
                    vt = v_ring.next()
                    K.dma("sp", [], [vt], lambda q: q.dma_start(out=vt[:], in_=VG[tk0:tk0 + 128, :]))
                    ggt = gg_ring2.next()
                    K.dma("sp", [], [ggt], lambda q: q.dma_start(out=ggt[:], in_=GG[tk0:tk0 + 128, :]))
                    rb = rb_ring.next()
                    K.dma("sp", [], [rb], lambda q: q.dma_start(out=rb[:, 0], in_=SF[cg]))
                    K.dma("sp", [], [rb], lambda q: q.dma_start(out=rb[:, 1], in_=RB[cg]))
                    return qk, vt, ggt, rb

                kT_ring = K.ring("na_k", [128, 4, 640], BF16, 2)
                qT_ring = K.ring("na_q", [128, 4, 128], BF16, 3)
                va_ring = K.ring("na_v", [128, 5, 520], BF16, 2)
                pT_ring = K.ring("na_p", [128, 5, 512], BF16, 2)
                on_ring = K.ring("na_o", [128, 512], BF16, 2)
                rs_ring = K.ring("na_rs", [128, 8], F32, 2)
                ont_ring = K.ring("na_ot", [128, 4, 128], BF16, 2)
                pairs = [(s0, T // GRID_W, r) for (s0, T) in cfg.seqs for r in range(0, T // GRID_W, 2)]

                def loadN(pi):
                    s0, rows, r = pairs[pi]
                    if 4 <= r <= rows - 6:
                        B_, nchk, tab, off = r - 4, 5, TI, None
                    else:
                        B_ = 0 if r < 4 else rows - 8
                        nchk, tab, off = 4, TB, B_ - r + 7
                    tq0 = s0 + r * 64
                    tkk = s0 + B_ * 64
                    kT = kT_ring.next()
                    K.dma("sp", [], [kT], lambda q: q.dma_start(out=kT[:, :, 0:nchk * 128], in_=NK.ap()[:, :, tkk:tkk + nchk * 128].rearrange("a p t -> p a t")))
                    qT = qT_ring.next()
                    K.dma("sp", [], [qT], lambda q: q.dma_start(out=qT[:], in_=NQ.ap()[:, :, tq0:tq0 + 128].rearrange("a p t -> p a t")))
                    va = va_ring.next()
                    K.dma("sp", [], [va], lambda q: q.dma_start(out=va[:, 0:nchk, :], in_=VN.ap()[tkk:tkk + nchk * 128, :].rearrange("(j p) c -> p j c", p=128)))
                    return nchk, tab, off, tq0, kT, qT, va

                xc_ring = K.ring("c_x", [128, D], F32, 3)
                h_ring = K.ring("c_h", [128, D], F32, 3)
                x1b_ring = K.ring("c_x1b", [128, D], BF16, 3)
                x1T_ring = K.ring("c_x1T", [128, 2, 8, 128], BF16, 2)
                xhl_ring = K.ring("c_xhl", [128, 2, D], BF16, 2)
                sm_ring = K.ring("c_sm", [128, 8, NE], F32, 3)
                mk_ring = K.ring("c_mk", [128, NE], BF16, 3)
                rc_ring = K.ring("c_rc", [128, 16], I32, 3)
                ident_f = cfs("ident")

                def loadC(ti):
                    r0 = ti * 128
                    xt = xc_ring.next()
                    K.dma("sp", [], [xt], lambda q: q.dma_start(out=xt[:], in_=xres[r0:r0 + 128, :]))
                    return xt

                def bodyO(cg, ld):
                    qk, vt, ggt, rb = ld
                    psum, psum_l = psO, plO
                    tk0 = cg * 128
                    po = psum_l.next()
                    for h in range(4):
                        m, hh = h // 2, h % 2
                        psl = slice(hh * 64, hh * 64 + 64)
                        ams = []
                        for d_ in range(2):
                            pa = psum.next()
                            K.op("pe", [qk], [pa], lambda pe: pe.matmul(pa[:, 0:128], qk[psl, d_ * 4 + 2 + m, :], qk[psl, d_ * 4 + m, :], start=True, stop=True))
                            am = am_ring.next()
                            if d_ == 0:
                                K.op("dve", [pa], [am], lambda v: v.tensor_tensor(am[:], pa[:, 0:128], tri_f, ALU.mult))
                            else:
                                K.op("dve", [pa], [am], lambda v: v.tensor_tensor(am[:], pa[:, 0:128], tri_b, ALU.mult))
                            ams.append(am)
                        osl = po[:, h * 128:(h + 1) * 128]
                        K.op("pe", [ams[0], vt], [po], lambda pe: pe.matmul(osl, ams[0][:], vt[:, h * 128:(h + 1) * 128], start=True, stop=False))
                        K.op("pe", [ams[1], vt], [po], lambda pe: pe.matmul(osl, ams[1][:], vt[:, h * 128:(h + 1) * 128], start=False, stop=False))
                        K.op("pe", [qk, rb], [po], lambda pe: pe.matmul(osl, qk[psl, m, :], rb[psl, 0, m, hh * 128:(hh + 1) * 128], start=False, stop=False))
                        K.op("pe", [qk, rb], [po], lambda pe: pe.matmul(osl, qk[psl, 4 + m, :], rb[psl, 1, m, hh * 128:(hh + 1) * 128], start=False, stop=True))
                    o = o_ring.next()
                    K.op("act", [po], [o], lambda a: a.copy(o[:], po[:, :]))
                    sq = o_ring.next()
                    K.op("pool", [o], [sq], lambda g: g.tensor_tensor(sq[:], o[:], o[:], ALU.mult))
                    stt = st_ring.next()
                    K.op("dve", [sq], [stt], lambda v: v.tensor_reduce(stt[:, 0:4], sq.t.ap().rearrange("p (h d) -> p h d", h=4), AX.X, ALU.add))
                    K.op("dve", [stt], [stt], lambda v: v.tensor_scalar(stt[:, 4:8], stt[:, 0:4], 1.0 / 128.0, RMS_EPS, ALU.mult, ALU.add))
                    K.op("pool", [stt], [stt], lambda g: g.tensor_tensor(stt[:, 8:12], stt[:, 4:8], cfs("neghalf"), ALU.pow))
                    sg = o_ring.next()
                    K.op("act", [ggt], [sg], lambda a: a.activation(sg[:], ggt[:], AF.Silu))
                    K.op("pool", [sg, normg], [sg], lambda g: g.tensor_tensor(sg[:], sg[:], normg[:], ALU.mult))
                    for h in range(4):
                        K.op("dve", [o, stt, sg], [o], lambda v: v.scalar_tensor_tensor(o[:, h * 128:(h + 1) * 128], o[:, h * 128:(h + 1) * 128], stt[:, 8 + h:9 + h], sg[:, h * 128:(h + 1) * 128], ALU.mult, ALU.mult))
                    ob = ob_ring.next()
                    K.op("act", [o], [ob], lambda a: a.copy(ob[:], o[:]))
                    pt = psum.next()
                    ptb = pt.t.ap().bitcast(BF16)
                    for k in range(4):
                        K.op("pe", [ob], [pt], lambda pe: pe.transpose(ptb[:, k * 128:(k + 1) * 128], ob[:, k * 128:(k + 1) * 128], ident_b))
                    ot = ot_ring.next()
                    K.op("act", [pt], [ot], lambda a: a.copy(ot.t.ap().rearrange("p k t -> p (k t)"), ptb[:, 0:512]))
                    return ot

                def bodyN(pi, ld):
                    nchk, tab, off, tq0, kT, qT, va = ld
                    psum, psum_l = psN, plN
                    on = on_ring.next()
                    rs = rs_ring.next()
                    for hg in range(2):
                        pT = pT_ring.next()
                        for j in range(nchk):
                            pst = psum.next()
                            for hh in range(4):
                                h = hg * 4 + hh
                                m = h // 2
                                psl = slice((h % 2) * 64, (h % 2) * 64 + 64)
                                if off is None:
                                    tsl = tab[:, h, j * 128:(j + 1) * 128]
                                else:
                                    tsl = tab[:, h, (off + 2 * j) * 64:(off + 2 * j + 2) * 64]
                                K.op("pe", [kT, qT], [pst], lambda pe: pe.matmul(pst[:, hh * 128:(hh + 1) * 128], kT[psl, m, j * 128:(j + 1) * 128], qT[psl, m, :], start=True, stop=False))
                                K.op("pe", [tab], [pst], lambda pe: pe.matmul(pst[:, hh * 128:(hh + 1) * 128], tsl, ident_b, start=False, stop=True))
                            K.op("act", [pst], [pT], lambda a: a.activation(pT[:, j, :], pst[:, :], AF.Exp))
                        po = psum_l.next()
                        for hh in range(4):
                            h = hg * 4 + hh
                            for j in range(nchk):
                                K.op("pe", [pT, va], [po], lambda pe: pe.matmul(po[:, hh * 65:(hh + 1) * 65], pT[:, j, hh * 128:(hh + 1) * 128], va[:, j, h * 65:(h + 1) * 65], start=(j == 0), stop=(j == nchk - 1)))
                        pov = po.t.ap()[:, 0:260].rearrange("p (h d) -> p h d", h=4)
                        K.op("dve", [po], [rs], lambda v: v.reciprocal(rs[:, hg * 4:(hg + 1) * 4], pov[:, :, 64]))
                        for hh in range(4):
                            h = hg * 4 + hh
                            K.op("dve", [po, rs], [on], lambda v: v.tensor_scalar(on[:, h * 64:(h + 1) * 64], po[:, hh * 65:hh * 65 + 64], rs[:, h:h + 1], None, ALU.mult))
                    pt = psum.next()
                    ptb = pt.t.ap().bitcast(BF16)
                    for k in range(4):
                        K.op("pe", [on], [pt], lambda pe: pe.transpose(ptb[:, k * 128:(k + 1) * 128], on[:, k * 128:(k + 1) * 128], ident_b))
                    ont = ont_ring.next()
                    K.op("act", [pt], [ont], lambda a: a.copy(ont.t.ap().rearrange("p k t -> p (k t)"), ptb[:, 0:512]))
                    return ont

                def stage1C(ti, ot, ont, xt):
                    r0 = ti * 128
                    psum = psC
                    ht = h_ring.next()
                    for n_ in range(2):
                        pm = psum.next()
                        for k in range(8):
                            K.op("pe", [ot, ont, w_out_sb], [pm], lambda pe: pe.matmul(pm[:, :], (ot if k < 4 else ont)[:, k % 4, :], w_out_sb[:, k, n_ * 512:(n_ + 1) * 512], start=(k == 0), stop=(k == 7)))
                        K.op("dve", [pm, xt], [ht], lambda v: v.scalar_tensor_tensor(ht[:, n_ * 512:(n_ + 1) * 512], xt[:, n_ * 512:(n_ + 1) * 512], cfg.alpha, pm[:, :], ALU.mult, ALU.add))
                    layer_norm(ht, ln1g, ln1b, ht, stats_ring)
                    K.dma("sp", [ht], [], lambda q: q.dma_start(out=X1[r0:r0 + 128, :], in_=ht[:]))
                    x1b = x1b_ring.next()
                    K.op("act", [ht], [x1b], lambda a: a.copy(x1b[:], ht[:]))
                    K.dma("sp", [x1b], [], lambda q: q.dma_start(out=X1B[r0:r0 + 128, :], in_=x1b[:]))
                    return ht

                def stage2C(ti, ht):
                    r0 = ti * 128
                    psum = psC
                    xhl = xhl_ring.next()
                    K.op("act", [ht], [xhl], lambda a: a.copy(xhl[:, 0, :], ht[:]))
                    K.op("dve", [ht, xhl], [xhl], lambda v: v.tensor_tensor(xhl[:, 1, :], ht[:], xhl[:, 0, :], ALU.subtract))
                    x1T = x1T_ring.next()
                    for part in range(2):
                        pt = psum.next()
                        ptb = pt.t.ap().bitcast(BF16)
                        for k in range(8):
                            K.op("pe", [xhl], [pt], lambda pe: pe.transpose(ptb[:, k * 128:(k + 1) * 128], xhl[:, part, k * 128:(k + 1) * 128], ident_b))
                        K.op("act", [pt], [x1T], lambda a: a.copy(x1T.t.ap()[:, part].rearrange("p k t -> p (k t)"), ptb[:, :]))
                    plg = psum.next()
                    combos = [(0, wr_hi), (0, wr_lo), (1, wr_hi)]
                    for ci, (part, wt) in enumerate(combos):
                        for k in range(8):
                            K.op("pe", [x1T, wt], [plg], lambda pe: pe.matmul(plg[:, 0:NE], x1T[:, part, k, :], wt[:, k, :], start=(ci == 0 and k == 0), stop=(ci == 2 and k == 7)))
                    sm = sm_ring.next()
                    lgs = sm[:, 0, :]
                    K.op("dve", [plg, br_bc], [sm], lambda v: v.tensor_tensor(lgs, plg[:, 0:NE], br_bc[:], ALU.add))
                    mx8 = sm[:, 1, 0:8]
                    K.op("dve", [sm], [sm], lambda v: v.max(mx8, lgs))
                    mk = mk_ring.next()
                    K.op("dve", [sm], [mk], lambda v: v.tensor_scalar(mk[:], lgs, sm[:, 1, 3:4], None, ALU.is_ge))
                    pps = psum.next()
                    K.op("pe", [mk], [pps], lambda pe: pe.matmul(pps[:, 0:NE], cbs("tri_s"), mk[:], start=True, stop=False))
                    K.op("pe", [mcum], [pps], lambda pe: pe.matmul(pps[:, 0:NE], cbs("ones"), mcum[:], start=False, stop=True))
                    K.op("pool", [mcum, mk], [mcum], lambda g: g.tensor_tensor(mcum[:], mcum[:], mk[:], ALU.add))
                    Dm = sm[:, 2, :]
                    K.op("dve", [pps], [sm], lambda v: v.tensor_tensor(Dm, pps[:, 0:NE], cfs("iotacap"), ALU.add))
                    ovf = sm[:, 3, :]
                    K.op("dve", [pps], [sm], lambda v: v.tensor_scalar(ovf, pps[:, 0:NE], float(CAP), float(NSLOT), ALU.is_ge, ALU.mult))
                    K.op("dve", [sm], [sm], lambda v: v.tensor_tensor(Dm, Dm, ovf, ALU.add))
                    negm = sm[:, 1, 8:9]
                    K.op("dve", [sm], [sm], lambda v: v.tensor_scalar(negm, sm[:, 1, 0:1], -1.0, None, ALU.mult))
                    ex4 = sm[:, 1, 12:16]
                    K.op("act", [sm], [sm], lambda a: a.activation(ex4, sm[:, 1, 0:4], AF.Exp, bias=negm, scale=1.0))
                    zz = sm[:, 1, 9:10]
                    K.op("dve", [sm], [sm], lambda v: v.tensor_reduce(zz, ex4, AX.X, ALU.add))
                    K.op("dve", [sm], [sm], lambda v: v.reciprocal(zz, zz))
                    rc = rc_ring.next()
                    rcf = rc.t.ap().bitcast(F32)
                    destf = sm[:, 1, 16:20]
                    tokf = sm[:, 1, 20:21]
                    K.op("dve", [], [sm], lambda v: v.tensor_scalar(tokf, cfs("pidx"), float(r0), None, ALU.add))
                    oh4 = sm.t.ap()[:, 4:8, :]
                    K.op("dve", [sm], [sm], lambda v: v.tensor_tensor(oh4, bass.AP(sm.t, 0, [[8 * NE, 128], [0, 4], [1, NE]]), bass.AP(sm.t, NE, [[8 * NE, 128], [1, 4], [0, NE]]), ALU.is_equal))
                    K.op("dve", [sm], [sm], lambda v: v.tensor_tensor(oh4, oh4, bass.AP(sm.t, 2 * NE, [[8 * NE, 128], [0, 4], [1, NE]]), ALU.mult))
                    K.op("dve", [sm], [sm], lambda v: v.tensor_reduce(destf, oh4, AX.X, ALU.add))
                    rc4 = rc.t.ap().rearrange("p (k f) -> p k f", f=4)
                    rcf4 = rcf.rearrange("p (k f) -> p k f", f=4)
                    K.op("dve", [sm], [rc], lambda v: v.tensor_copy(rc4[:, :, 0], bass.AP(sm.t, NE + 20, [[8 * NE, 128], [0, 4]])))
                    K.op("dve", [sm], [rc], lambda v: v.tensor_scalar(rcf4[:, :, 1], ex4, zz, None, ALU.mult))
                    K.op("dve", [sm], [rc], lambda v: v.scalar_tensor_tensor(rc4[:, :, 2], bass.AP(sm.t, NE + 20, [[8 * NE, 128], [0, 4]]), 4.0, cfs("k0123"), ALU.mult, ALU.add))
                    isov = sm[:, 1, 24:28]
                    K.op("dve", [sm], [sm], lambda v: v.tensor_scalar(isov, destf, float(NSLOT), None, ALU.is_ge))
                    K.op("dve", [sm], [sm], lambda v: v.tensor_scalar(sm[:, 1, 28:32], isov, -1.0, 1.0, ALU.mult, ALU.add))
                    K.op("dve", [sm], [sm], lambda v: v.tensor_tensor(destf, destf, sm[:, 1, 28:32], ALU.mult))
                    K.op("dve", [sm], [sm], lambda v: v.tensor_scalar(isov, isov, cfs("dummyslot"), None, ALU.mult))
                    K.op("dve", [sm], [sm], lambda v: v.tensor_tensor(destf, destf, isov, ALU.add))
                    K.op("dve", [sm], [rc], lambda v: v.tensor_copy(rc4[:, :, 3], destf))
                    pass
                    for k in range(4):
                        K.dma("pool", [rc], [REC_tk] if cfg.serial_scatter else [], lambda q: q.indirect_dma_start(
                            out=REC.ap()[:, :], out_offset=bass.IndirectOffsetOnAxis(ap=rc[:, 4 * k + 3:4 * k + 4], axis=0),
                            in_=rc[:, 4 * k:4 * k + 4], in_offset=None))

                psO, plO = Ring(psum_all[0:2]), Ring(psum_all[6:7])
                psN, plN = Ring(psum_all[2:4]), Ring(psum_all[7:8])
                psC = Ring(psum_all[4:6])
                coop = Coop(K)
                pO, pN, pC = [loadO(0)], [loadN(0)], [loadC(0)]
                res = {}
                hts = {}
                for i in range(NT + 2):
                    if i + 1 < NT:
                        pO.append(loadO(i + 1))
                        pN.append(loadN(i + 1))
                        pC.append(loadC(i + 1))
                    fns = []
                    if i < NT:
                        ldO, ldN = pO.pop(0), pN.pop(0)
                        fns.append(lambda: res.__setitem__(("o", i), bodyO(i, ldO)))
                        fns.append(lambda: res.__setitem__(("n", i), bodyN(i, ldN)))

                    def cstream():
                        if 1 <= i <= NT:
                            hts[i - 1] = stage1C(i - 1, res.pop(("o", i - 1)), res.pop(("n", i - 1)), pC.pop(0))
                        if 2 <= i <= NT + 1:
                            stage2C(i - 2, hts.pop(i - 2))
                    fns.append(cstream)
                    coop.run(fns)
                K.barrier()
                K.es = old_es
            if cfg.debug == "C":
                break
            with ExitStack() as esD:
                old_es = K.es
                K.es = esD
                wgu_ring = K.ring("d_wgu", [128, 8, 2 * D], BF16, 2)
                wd_ring = K.ring("d_wd", [128, 8, D], BF16, 2)
                bgu_ring = K.ring("d_bgu", [128, 16], F32, 2)
                bd_ring = K.ring("d_bd", [1, D], BF16, 2)
                rec_ring = K.ring("d_rec", [128, 4], I32, 12)
                xg_ring = K.ring("d_xg", [128, D], BF16, 8)
                xsT_ring = K.ring("d_xsT", [128, 8, 512], BF16, 2)
                hT_ring = K.ring("d_hT", [128, 8, 512], BF16, 2)
                t_ring = K.ring("d_t", [128, 512], F32, 6)
                ys_ring = K.ring("d_ys", [128, D], F32, 3)
                GT = CAP // 128

                def load_w(e):
                    wgu = wgu_ring.next()
                    wd = wd_ring.next()
                    bgu = bgu_ring.next()
                    bd = bd_ring.next()
                    K.dma("pool", [], [wgu], lambda q: q.dma_start(out=wgu[:], in_=W["w_gu"][l, e].rearrange("(k p) c -> p k c", p=128)))
                    K.dma("pool", [], [wd], lambda q: q.dma_start(out=wd[:], in_=W["w_down"][l, e].rearrange("(k p) c -> p k c", p=128)))
                    K.dma("sp", [], [bgu], lambda q: q.dma_start(out=bgu[:], in_=W["b_gu"][l, e].rearrange("(m p) -> p m", p=128), allow_slow_non_contiguous=True))
                    K.dma("pool", [], [bd], lambda q: q.dma_start(out=bd[:], in_=W["b_down"][l, e:e + 1, :]))
                    return wgu, wd, bgu, bd

                groups = [(e, g0, min(4, GT - g0)) for e in range(NE) for g0 in range(0, GT, 4)]

                def prep_loads(gi):
                    e, g0, ng = groups[gi]
                    recs, xgs = [], []
                    for j in range(ng):
                        slot0 = e * CAP + (g0 + j) * 128
                        rec = rec_ring.next()
                        K.dma("sp", [], [rec], lambda q: q.dma_start(out=rec[:], in_=REC.ap()[slot0:slot0 + 128, :]))
                        recs.append(rec)
                        xg = xg_ring.next()
                        K.dma("pool", [rec], [xg], lambda q: q.indirect_dma_start(
                            out=xg[:, :], out_offset=None, in_=X1B.ap()[:, :],
                            in_offset=bass.IndirectOffsetOnAxis(ap=rec[:, 0:1], axis=0)))
                        xgs.append(xg)
                    return recs, xgs

                def prep_T(xgs):
                    xsT = xsT_ring.next()
                    for j, xg in enumerate(xgs):
                        pt = psum.next()
                        ptb = pt.t.ap().bitcast(BF16)
                        for k in range(8):
                            K.op("pe", [xg], [pt], lambda pe: pe.transpose(ptb[:, k * 128:(k + 1) * 128], xg[:, k * 128:(k + 1) * 128], ident_b))
                        K.op("act", [pt], [xsT], lambda a: a.copy(xsT[:, :, j * 128:(j + 1) * 128], ptb.rearrange("p (k t) -> p k t", k=8)))
                    return xsT

                nxt = load_w(0)
                recs, xgs = prep_loads(0)
                xsT = prep_T(xgs)
                for gi, (e, g0, ng) in enumerate(groups):
                    if g0 == 0:
                        wgu, wd, bgu, bd = nxt
                        if e + 1 < NE:
                            nxt = load_w(e + 1)
                    W_ = ng * 128
                    hT = hT_ring.next()
                    nrecs = nxgs = None
                    for m in range(8):
                        if m == 4 and gi + 1 < len(groups):
                            nrecs, nxgs = prep_loads(gi + 1)
                        pg = psum.next()
                        for k in range(8):
                            K.op("pe", [xsT, wgu], [pg], lambda pe: pe.matmul(pg[:, 0:W_], wgu[:, k, m * 128:(m + 1) * 128], xsT[:, k, 0:W_], start=(k == 0), stop=(k == 7)))
                        pu = psum.next()
                        for k in range(8):
                            K.op("pe", [xsT, wgu], [pu], lambda pe: pe.matmul(pu[:, 0:W_], wgu[:, k, D + m * 128:D + (m + 1) * 128], xsT[:, k, 0:W_], start=(k == 0), stop=(k == 7)))
                        g1 = t_ring.next()
                        K.op("dve", [pg, bgu], [g1], lambda v: v.tensor_scalar(g1[:, 0:W_], pg[:, 0:W_], bgu[:, m:m + 1], SW_LIM, ALU.add, ALU.min))
                        sg = t_ring.next()
                        K.op("act", [g1], [sg], lambda a: a.activation(sg[:, 0:W_], g1[:, 0:W_], AF.Sigmoid, scale=SW_ALPHA))
                        u1 = t_ring.next()
                        K.op("dve", [pu, bgu], [u1], lambda v: v.tensor_scalar(u1[:, 0:W_], pu[:, 0:W_], bgu[:, 8 + m:9 + m], SW_LIM, ALU.add, ALU.min))
                        K.op("dve", [u1], [u1], lambda v: v.tensor_scalar(u1[:, 0:W_], u1[:, 0:W_], -SW_LIM, 1.0, ALU.max, ALU.add))
                        K.op("pool", [g1, sg], [g1], lambda g: g.tensor_tensor(g1[:, 0:W_], g1[:, 0:W_], sg[:, 0:W_], ALU.mult))
                        K.op("dve", [g1, u1], [hT], lambda v: v.tensor_tensor(hT[:, m, 0:W_], g1[:, 0:W_], u1[:, 0:W_], ALU.mult))
                    nxsT = prep_T(nxgs) if nxgs is not None else None
                    for j in range(ng):
                        rec = recs[j]
                        recf = rec.t.ap().bitcast(F32)
                        ys = ys_ring.next()
                        for n_ in range(2):
                            py = psum.next()
                            for m in range(8):
                                K.op("pe", [hT, wd], [py], lambda pe: pe.matmul(py[:, :], hT[:, m, j * 128:(j + 1) * 128], wd[:, m, n_ * 512:(n_ + 1) * 512], start=(m == 0), stop=False))
                            K.op("pe", [bd], [py], lambda pe: pe.matmul(py[:, :], cb[0:1, CO["b"]["ones"][0]:CO["b"]["ones"][0] + 128], bd[:, n_ * 512:(n_ + 1) * 512], start=False, stop=True))
                            K.op("act", [py, rec], [ys], lambda a: a.activation(ys[:, n_ * 512:(n_ + 1) * 512], py[:, :], AF.Copy, scale=recf[:, 1:2]))
                        K.dma("pool", [ys, rec], [YS_tk] if cfg.serial_scatter else [], lambda q: q.indirect_dma_start(
                            out=YS.ap()[:, :], out_offset=bass.IndirectOffsetOnAxis(ap=rec[:, 2:3], axis=0),
                            in_=ys[:, :], in_offset=None))
                    recs, xgs, xsT = nrecs, nxgs, nxsT
                K.barrier()
                K.es = old_es
            if cfg.debug == "D":
                break
            with ExitStack() as esE:
                old_es = K.es
                K.es = esE
                wpg = K.sb("e_wpg", [128, 8, D], BF16)
                K.dma("pool", [], [wpg], lambda q: q.dma_start(out=wpg[:], in_=W["w_ple_gate"][l].rearrange("(k p) c -> p k c", p=128)))
                wpp = K.sb("e_wpp", [128, 2, D], BF16)
                K.dma("pool", [], [wpp], lambda q: q.dma_start(out=wpp[:], in_=W["w_ple_proj"][l].rearrange("(k p) c -> p k c", p=128)))
                bpg = K.sb("e_bpg", [1, D], BF16)
                K.dma("pool", [], [bpg], lambda q: q.dma_start(out=bpg[:], in_=W["b_ple_gate"][l:l + 1, :]))
                ln2g = bcast_load("ln2g", W["ln2_g"][l, :], D)
                ln2b = bcast_load("ln2b", W["ln2_b"][l, :], D)
                K.barrier()
                x1_ring = K.ring("e_x1", [128, D], F32, 4)
                ys4_ring = K.ring("e_ys4", [128, 4, D], F32, 4)
                p_ring = K.ring("e_p", [128, PLE], F32, 4)
                pb_ring = K.ring("e_pb", [128, PLE], BF16, 4)
                rb_ring2 = K.ring("e_rb", [128, D], BF16, 4)
                rT_ring = K.ring("e_rT", [128, 8, 128], BF16, 4)
                pT_ring2 = K.ring("e_pT", [128, 2, 128], BF16, 4)
                sg_ring = K.ring("e_sg", [128, 512], F32, 6)
                r2_ring = K.ring("e_r2", [128, D], F32, 4)
                dst = xres if l + 1 < L else y_out
                def loadE(ti):
                    r0 = ti * 128
                    x1t = x1_ring.next()
                    K.dma("sp", [], [x1t], lambda q: q.dma_start(out=x1t[:], in_=X1[r0:r0 + 128, :]))
                    ys4 = ys4_ring.next()
                    K.dma("sp", [], [ys4], lambda q: q.dma_start(out=ys4[:], in_=YS.ap()[4 * r0:4 * r0 + 512, :].rearrange("(p k) c -> p k c", k=4)))
                    pt_ = p_ring.next()
                    K.dma("sp", [], [pt_], lambda q: q.dma_start(out=pt_[:], in_=p_in[l, r0:r0 + 128, :]))
                    return x1t, ys4, pt_

                def bodyE(ti, ld, psr2):
                    pst_, psr = psr2
                    x1t, ys4, pt_ = ld
                    r0 = ti * 128
                    K.op("dve", [x1t, ys4], [x1t], lambda v: v.scalar_tensor_tensor(x1t[:], x1t[:], cfg.alpha, ys4[:, 0, :], ALU.mult, ALU.add))
                    K.op("pool", [x1t, ys4], [x1t], lambda g: g.tensor_tensor(x1t[:], x1t[:], ys4[:, 1, :], ALU.add))
                    K.op("dve", [x1t, ys4], [x1t], lambda v: v.tensor_tensor(x1t[:], x1t[:], ys4[:, 2, :], ALU.add))
                    K.op("pool", [x1t, ys4], [x1t], lambda g: g.tensor_tensor(x1t[:], x1t[:], ys4[:, 3, :], ALU.add))
                    rb_ = rb_ring2.next()
                    K.op("act", [x1t], [rb_], lambda a: a.copy(rb_[:], x1t[:]))
                    pb_ = pb_ring.next()
                    K.op("act", [pt_], [pb_], lambda a: a.copy(pb_[:], pt_[:]))
                    ptr = pst_.next()
                    ptrb = ptr.t.ap().bitcast(BF16)
                    for k in range(8):
                        K.op("pe", [rb_], [ptr], lambda pe: pe.transpose(ptrb[:, k * 128:(k + 1) * 128], rb_[:, k * 128:(k + 1) * 128], ident_b))
                    rT = rT_ring.next()
                    K.op("dve", [ptr], [rT], lambda v: v.tensor_copy(rT.t.ap().rearrange("p k t -> p (k t)"), ptrb[:, :]))
                    ptp = pst_.next()
                    ptpb = ptp.t.ap().bitcast(BF16)
                    for k in range(2):
                        K.op("pe", [pb_], [ptp], lambda pe: pe.transpose(ptpb[:, k * 128:(k + 1) * 128], pb_[:, k * 128:(k + 1) * 128], ident_b))
                    pT = pT_ring2.next()
                    K.op("act", [ptp], [pT], lambda a: a.copy(pT.t.ap().rearrange("p k t -> p (k t)"), ptpb[:, 0:256]))
                    r2 = r2_ring.next()
                    for n_ in range(2):
                        pgt = psr.next()
                        for k in range(8):
                            K.op("pe", [rT, wpg], [pgt], lambda pe: pe.matmul(pgt[:, :], rT[:, k, :], wpg[:, k, n_ * 512:(n_ + 1) * 512], start=(k == 0), stop=False))
                        K.op("pe", [bpg], [pgt], lambda pe: pe.matmul(pgt[:, :], cb[0:1, CO["b"]["ones"][0]:CO["b"]["ones"][0] + 128], bpg[:, n_ * 512:(n_ + 1) * 512], start=False, stop=True))
                        ppj = psr.next()
                        for k in range(2):
                            K.op("pe", [pT, wpp], [ppj], lambda pe: pe.matmul(ppj[:, :], pT[:, k, :], wpp[:, k, n_ * 512:(n_ + 1) * 512], start=(k == 0), stop=(k == 1)))
                        sg = sg_ring.next()
                        K.op("act", [pgt], [sg], lambda a: a.activation(sg[:], pgt[:, :], AF.Sigmoid))
                        K.op("dve", [sg, ppj], [sg], lambda v: v.tensor_tensor(sg[:], sg[:], ppj[:, :], ALU.mult))
                        K.op("pool", [sg, x1t], [r2], lambda g: g.tensor_tensor(r2[:, n_ * 512:(n_ + 1) * 512], x1t[:, n_ * 512:(n_ + 1) * 512], sg[:], ALU.add))
                    layer_norm(r2, ln2g, ln2b, r2, stats_ring)
                    K.dma("sp", [r2], [], lambda q: q.dma_start(out=dst[r0:r0 + 128, :], in_=r2[:]))

                NS_E = 2 if NT % 2 == 0 else 1
                psE = [(Ring(psum_all[0:2]), Ring([psum_all[2], psum_all[6]])), (Ring(psum_all[3:5]), Ring([psum_all[5], psum_all[7]]))][:NS_E]
                coopE = Coop(K)
                pendE = [loadE(t_) for t_ in range(min(NS_E, NT))]
                for t0_ in range(0, NT, NS_E):
                    for t_ in range(t0_ + NS_E, min(t0_ + 2 * NS_E, NT)):
                        pendE.append(loadE(t_))
                    cur = [pendE.pop(0) for _ in range(NS_E)]
                    coopE.run([(lambda j=j: bodyE(t0_ + j, cur[j], psE[j])) for j in range(NS_E)])
                K.barrier()
                K.es = old_es

        K.barrier()
        K.finish()
    print("instructions:", K.ninst)
    return nc


def const_offsets(cfg):
    f = {}
    o = 0
    for name, w in (("eps_ln", 1), ("one", 1), ("eps_rms", 1), ("segmask", 512), ("ident", 128), ("onesf", 128), ("pidx", 1), ("iotacap", NE), ("dummyslot", 1), ("neghalf", 4), ("k0123", 4)):
        f[name] = (o, w)
        o += w
    fw = o
    b = {}
    o = 0
    for name, w in (("ident", 128), ("ones", 128), ("tri_f", 128), ("tri_b", 128), ("tri_s", 128)):
        b[name] = (o, w)
        o += w
    return {"f": f, "b": b, "fw": fw, "bw": o}


def const_shapes(cfg):
    co = const_offsets(cfg)
    return [128, co["fw"]], [128, co["bw"]]


def make_consts(cfg):
    co = const_offsets(cfg)
    cf = np.zeros((128, co["fw"]), np.float32)
    cbf = np.zeros((128, co["bw"]), np.float32)

    def setf(name, v):
        o, w = co["f"][name]
        cf[:, o:o + w] = v

    def setb(name, v):
        o, w = co["b"][name]
        cbf[:, o:o + w] = v

    setf("eps_ln", LN_EPS)
    setf("one", 1.0)
    setf("eps_rms", RMS_EPS)
    sm = np.ones((128, 512), np.float32)
    sm[:, 0::128] = 0.0
    setf("segmask", sm)
    setf("ident", np.eye(128, dtype=np.float32))
    setf("onesf", 1.0)
    setf("pidx", np.arange(128, dtype=np.float32)[:, None])
    setf("neghalf", -0.5)
    setf("k0123", np.arange(4, dtype=np.float32)[None, :])
    setf("dummyslot", (NE * cfg.cap + np.arange(128, dtype=np.float32))[:, None])
    setf("iotacap", (np.arange(NE, dtype=np.float32) * cfg.cap)[None, :])
    setb("ident", np.eye(128, dtype=np.float32))
    setb("ones", 1.0)
    j = np.arange(128)[:, None]
    i = np.arange(128)[None, :]
    setb("tri_f", (j <= i).astype(np.float32))
    setb("tri_b", (j > i).astype(np.float32))
    setb("tri_s", (j < i).astype(np.float32))
    return cf, cbf.astype(ml_dtypes.bfloat16)


def na_masks():
    p = np.arange(128)
    i_ = (p // 64)[:, None, None]
    c = (p % 64)[:, None, None]
    w = np.arange(64)[None, None, :]
    cs = np.clip(c - 8, 0, 48)
    colok = (w >= cs) & (w < cs + 16)
    mu = np.arange(16)[None, :, None]
    mB = np.where(colok & (mu - i_ >= 0) & (mu - i_ <= 14), 0.0, NEG).astype(np.float32).reshape(128, 1024)
    mm = np.arange(10)[None, :, None]
    mI = np.where(colok & (mm - i_ >= 0) & (mm - i_ <= 7), 0.0, NEG).astype(np.float32).reshape(128, 640)
    return mB, mI


def core_inputs(cfg, inputs, bp, bs, cf, cb):
    m = {}
    m["x_in"] = np.ascontiguousarray(np.concatenate([inputs["x_prompt"][bp], inputs["x_sample"][bs]], axis=0))
    m["p_in"] = np.ascontiguousarray(np.concatenate([inputs["p_prompt"][:cfg.depth, bp], inputs["p_sample"][:cfg.depth, bs]], axis=1))
    for k in ("w_in", "w_gk_f", "b_gk_f", "w_gk_b", "b_gk_b", "gla_norm_g", "rpb", "w_out", "ln1_g", "ln1_b",
              "w_router", "b_router", "w_gu", "b_gu", "w_down", "b_down", "w_ple_proj", "w_ple_gate",
              "b_ple_gate", "ln2_g", "ln2_b"):
        m[k] = np.asarray(inputs[k])[:cfg.depth]
    m["emb_ln_g"] = np.asarray(inputs["emb_ln_g"]).reshape(1, D)
    m["emb_ln_b"] = np.asarray(inputs["emb_ln_b"]).reshape(1, D)
    m["cf"] = cf
    m["cb"] = cb
    m["namb"], m["nami"] = na_masks()
    ri = np.zeros((128, 128, 4), np.int32)
    ri[:, :, 0] = cfg.N
    ri[:, :, 2] = 4 * cfg.N + np.arange(128)[None, :]
    m["reci"] = ri
    return m


def kernel(**inputs):
    cfg = Cfg()
    nc = build(cfg)
    cf, cb = make_consts(cfg)
    n = 8
    in_maps = [core_inputs(cfg, inputs, c, c % 4, cf, cb) for c in range(n)]
    res = run_bass_kernel_spmd(nc, in_maps, core_ids=list(range(n)))
    outs = [np.asarray(r["y_out"]) for r in res.results]
    y_prompt = np.stack([outs[c][:cfg.Tp] for c in range(8)], axis=0).astype(np.float32)
    y_sample = np.stack([outs[c][cfg.Tp:] for c in range(4)], axis=0).astype(np.float32)
    return (y_prompt, y_sample)
```
